# Optimizing a Trainium2 kernel written in Bass

```python
import math, functools
import jax, jax.numpy as jnp
from jax import lax
import numpy as np

D_MODEL = 4096
BATCH = 2
SEQ = 8192
DEPTH = 4

GRID_W = 64
CTX_LEN = 256
N_MIXERS = 2
Q_BLOCK = 128
ROPE_BASE = 10000.0
EPS = 1e-6
N_MOD = 6

MLA_HEADS = 32
MLA_Q_RANK = 1024
MLA_KV_RANK = 512
MLA_NOPE_DIM = 128
MLA_ROPE_DIM = 64
MLA_V_DIM = 128
MLA_QK_DIM = MLA_NOPE_DIM + MLA_ROPE_DIM
MLA_SCALE = MLA_QK_DIM ** -0.5

DIFF_HEAD_DIM = 128
DIFF_HEADS = D_MODEL // (2 * DIFF_HEAD_DIM)
DIFF_SCALE = DIFF_HEAD_DIM ** -0.5

FFN_DIM = 3072
N_EXPERTS = 8
TOP_K = 2
EXPERT_DIM = 1024

kernel_name = "hybrid_mla_diffattn_moe_dit"


def rms_norm(x, g):
    xf = x.astype(jnp.float32)
    y = xf * lax.rsqrt(jnp.mean(xf * xf, axis=-1, keepdims=True) + EPS)
    return (y * g.astype(jnp.float32)).astype(x.dtype)


def modulate(x, g, shift, scale):
    return rms_norm(x, g) * (1 + scale) + shift


def rope_tables(pos, dim):
    inv = ROPE_BASE ** (-jnp.arange(0, dim, 2, dtype=jnp.float32) / dim)
    ang = pos[:, None] * inv[None, :]
    return jnp.cos(ang), jnp.sin(ang)


def rope_rotate(x, cos, sin):
    shape = (cos.shape[0],) + (1,) * (x.ndim - 3) + (cos.shape[1],)
    cos = cos.reshape(shape).astype(x.dtype)
    sin = sin.reshape(shape).astype(x.dtype)
    x1, x2 = jnp.split(x, 2, axis=-1)
    return jnp.concatenate([x1 * cos - x2 * sin, x2 * cos + x1 * sin], axis=-1)


def axial_rope(x, row, col):
    half = x.shape[-1] // 2
    cr, sr = rope_tables(row, half)
    cc, scol = rope_tables(col, half)
    return jnp.concatenate([rope_rotate(x[..., :half], cr, sr),
                            rope_rotate(x[..., half:], cc, scol)], axis=-1)


def map_query_blocks(fn, queries):
    B, T = queries[0].shape[:2]
    nb = T // Q_BLOCK
    blocks = tuple(jnp.moveaxis(q.reshape((B, nb, Q_BLOCK) + q.shape[2:]), 1, 0) for q in queries)
    out = lax.map(lambda blk: fn(*blk), blocks)
    out = jnp.moveaxis(out, 0, 1)
    return out.reshape((B, T) + out.shape[3:])


def mla_queries(h, w_dq, q_norm, w_uq, rope):
    B, T, _ = h.shape
    q = (rms_norm(h @ w_dq, q_norm) @ w_uq).reshape(B, T, MLA_HEADS, MLA_QK_DIM)
    q_nope, q_pe = jnp.split(q, [MLA_NOPE_DIM], axis=-1)
    if rope is not None:
        q_pe = rope(q_pe)
    return q_nope, q_pe


def mla_keys_values(h, w_dkv, kv_norm, w_ukv, rope):
    B, T, _ = h.shape
    c_kv, k_pe = jnp.split(h @ w_dkv, [MLA_KV_RANK], axis=-1)
    kv = (rms_norm(c_kv, kv_norm) @ w_ukv).reshape(B, T, MLA_HEADS, MLA_NOPE_DIM + MLA_V_DIM)
    k_nope, v = jnp.split(kv, [MLA_NOPE_DIM], axis=-1)
    if rope is not None:
        k_pe = rope(k_pe)
    return k_nope, k_pe, v


def mla_attend(q_nope, q_pe, k_nope, k_pe, v):
    s = (jnp.einsum('bqhd,bkhd->bhqk', q_nope, k_nope)
         + jnp.einsum('bqhr,bkr->bhqk', q_pe, k_pe))
    p = jax.nn.softmax(s.astype(jnp.float32) * MLA_SCALE, axis=-1).astype(v.dtype)
    return jnp.einsum('bhqk,bkhd->bqhd', p, v)


def mla_mixer(h_lat, h_ctx, w_dq, q_norm, w_uq, w_dkv, kv_norm, w_ukv, w_o, rope, with_ctx_out):
    B, S, _ = h_lat.shape
    C = h_ctx.shape[1]
    kc, kpc, vc = mla_keys_values(h_ctx, w_dkv, kv_norm, w_ukv, None)
    kl, kpl, vl = mla_keys_values(h_lat, w_dkv, kv_norm, w_ukv, rope)
    k_all = jnp.concatenate([kc, kl], axis=1)
    kp_all = jnp.concatenate([kpc, kpl], axis=1)
    v_all = jnp.concatenate([vc, vl], axis=1)
    ql, qpl = mla_queries(h_lat, w_dq, q_norm, w_uq, rope)
    o_lat = map_query_blocks(lambda qn, qp: mla_attend(qn, qp, k_all, kp_all, v_all), (ql, qpl))
    y_lat = o_lat.reshape(B, S, MLA_HEADS * MLA_V_DIM) @ w_o
    if not with_ctx_out:
        return y_lat, None
    qc, qpc = mla_queries(h_ctx, w_dq, q_norm, w_uq, None)
    y_ctx = mla_attend(qc, qpc, kc, kpc, vc).reshape(B, C, MLA_HEADS * MLA_V_DIM) @ w_o
    return y_lat, y_ctx


def diff_queries(h, w_q, rope):
    B, T, _ = h.shape
    q = (h @ w_q).reshape(B, T, DIFF_HEADS, 2, DIFF_HEAD_DIM)
    return q if rope is None else rope(q)


def diff_keys_values(h, w_kv, rope):
    B, T, _ = h.shape
    k, v = jnp.split(h @ w_kv, 2, axis=-1)
    k = k.reshape(B, T, DIFF_HEADS, 2, DIFF_HEAD_DIM)
    v = v.reshape(B, T, DIFF_HEADS, 2 * DIFF_HEAD_DIM)
    if rope is not None:
        k = rope(k)
    return k, v


def diff_attend(q, k, v, lam):
    s = jnp.einsum('bqhnd,bkhnd->bhnqk', q, k).astype(jnp.float32) * DIFF_SCALE
    p = jax.nn.softmax(s, axis=-1)
    a = (p[:, :, 0] - lam * p[:, :, 1]).astype(v.dtype)
    return jnp.einsum('bhqk,bkhe->bqhe', a, v)


def diff_mixer(h_lat, h_ctx, w_qkv, lam_params, subln, w_o, lambda_init, rope, with_ctx_out):
    B, S, D = h_lat.shape
    w_q, w_kv = w_qkv[:, :D], w_qkv[:, D:]
    lp = lam_params.astype(jnp.float32)
    lam = jnp.exp(jnp.sum(lp[0] * lp[1])) - jnp.exp(jnp.sum(lp[2] * lp[3])) + lambda_init
    kc, vc = diff_keys_values(h_ctx, w_kv, None)
    kl, vl = diff_keys_values(h_lat, w_kv, rope)
    k_all = jnp.concatenate([kc, kl], axis=1)
    v_all = jnp.concatenate([vc, vl], axis=1)

    def finish(o):
        o = rms_norm(o, subln) * (1.0 - lambda_init)
        return o.reshape(o.shape[0], o.shape[1], D) @ w_o

    ql = diff_queries(h_lat, w_q, rope)
    y_lat = finish(map_query_blocks(lambda q: diff_attend(q, k_all, v_all, lam), (ql,)))
    if not with_ctx_out:
        return y_lat, None
    qc = diff_queries(h_ctx, w_q, None)
    y_ctx = finish(diff_attend(qc, kc, vc, lam))
    return y_lat, y_ctx


def swiglu(h, w_gu, w_down):
    g, u = jnp.split(h @ w_gu, 2, axis=-1)
    return (jax.nn.silu(g) * u) @ w_down


def moe_ffn(h, w_router, w_gu, w_down):
    logits = (h @ w_router).astype(jnp.float32)
    top_val, top_idx = lax.top_k(logits, TOP_K)
    top_w = jax.nn.softmax(top_val, axis=-1)
    combine = jnp.sum(jax.nn.one_hot(top_idx, N_EXPERTS, dtype=jnp.float32) * top_w[..., None], axis=-2)
    y = jnp.zeros_like(h)
    for e in range(N_EXPERTS):
        y = y + combine[..., e:e + 1].astype(h.dtype) * swiglu(h, w_gu[e], w_down[e])
    return y


def setup_inputs(seed: int = 0) -> dict:
    key = jax.random.key(seed)
    ks = iter(jax.random.split(key, 24))
    L, La, Lb = DEPTH, (DEPTH + 1) // 2, DEPTH // 2
    D = D_MODEL

    def w(shape, fan_in, gain=1.0):
        return jax.random.normal(next(ks), shape, jnp.float32) * (gain * fan_in ** -0.5)

    def gains(shape):
        return 1.0 + 0.02 * jax.random.normal(next(ks), shape, jnp.float32)

    return {
        "x": jax.random.normal(next(ks), (BATCH, SEQ, D), jnp.float32),
        "c": jax.random.normal(next(ks), (BATCH, D), jnp.float32),
        "ctx": jax.random.normal(next(ks), (BATCH, CTX_LEN, D), jnp.float32),
        "c_ctx": jax.random.normal(next(ks), (D,), jnp.float32),
        "ada_w": w((L, D, N_MOD * D), D, 0.5),
        "ada_b": 0.02 * jax.random.normal(next(ks), (L, N_MOD * D), jnp.float32),
        "norm_g": gains((L, 4, D)),
        "mla_w_dq": w((La, D, MLA_Q_RANK), D),
        "mla_q_norm": gains((La, MLA_Q_RANK)),
        "mla_w_uq": w((La, MLA_Q_RANK, MLA_HEADS * MLA_QK_DIM), MLA_Q_RANK),
        "mla_w_dkv": w((La, D, MLA_KV_RANK + MLA_ROPE_DIM), D),
        "mla_kv_norm": gains((La, MLA_KV_RANK)),
        "mla_w_ukv": w((La, MLA_KV_RANK, MLA_HEADS * (MLA_NOPE_DIM + MLA_V_DIM)), MLA_KV_RANK),
        "mla_w_o": w((La, MLA_HEADS * MLA_V_DIM, D), MLA_HEADS * MLA_V_DIM),
        "diff_w_qkv": w((Lb, D, 3 * D), D),
        "diff_lambda": 0.1 * jax.random.normal(next(ks), (Lb, 4, DIFF_HEAD_DIM), jnp.float32),
        "diff_subln": gains((Lb, 2 * DIFF_HEAD_DIM)),
        "diff_w_o": w((Lb, D, D), D),
        "ffn_w_gu": w((La, D, 2 * FFN_DIM), D),
        "ffn_w_down": w((La, FFN_DIM, D), FFN_DIM),
        "moe_router": w((Lb, D, N_EXPERTS), D),
        "moe_w_gu": w((Lb, N_EXPERTS, D, 2 * EXPERT_DIM), D),
        "moe_w_down": w((Lb, N_EXPERTS, EXPERT_DIM, D), EXPERT_DIM),
    }


def reference(x, c, ctx, c_ctx, ada_w, ada_b, norm_g,
              mla_w_dq, mla_q_norm, mla_w_uq, mla_w_dkv, mla_kv_norm, mla_w_ukv, mla_w_o,
              diff_w_qkv, diff_lambda, diff_subln, diff_w_o,
              ffn_w_gu, ffn_w_down, moe_router, moe_w_gu, moe_w_down):
    B, S, D = x.shape
    C = ctx.shape[1]
    rows = S // GRID_W
    row = jnp.repeat(jnp.arange(rows, dtype=jnp.float32), GRID_W)
    col = jnp.tile(jnp.arange(GRID_W, dtype=jnp.float32), rows)
    rope = functools.partial(axial_rope, row=row, col=col)

    silu_c = jax.nn.silu(c)
    silu_cc = jax.nn.silu(c_ctx)
    xl, xc = x, ctx
    for i in range(DEPTH):
        j = i // 2
        need_ctx = i < DEPTH - 1
        g = norm_g[i]
        mod_l = (silu_c @ ada_w[i] + ada_b[i]).reshape(B, N_MOD, 1, D)
        mod_c = (silu_cc @ ada_w[i] + ada_b[i]).reshape(N_MOD, D)

        h_l = modulate(xl, g[0], mod_l[:, 0], mod_l[:, 1])
        h_c = modulate(xc, g[0], mod_c[0], mod_c[1])
        if i % N_MIXERS == 0:
            y_l, y_c = mla_mixer(h_l, h_c, mla_w_dq[j], mla_q_norm[j], mla_w_uq[j], mla_w_dkv[j],
                                 mla_kv_norm[j], mla_w_ukv[j], mla_w_o[j], rope, need_ctx)
        else:
            lambda_init = 0.8 - 0.6 * math.exp(-0.3 * i)
            y_l, y_c = diff_mixer(h_l, h_c, diff_w_qkv[j], diff_lambda[j], diff_subln[j], diff_w_o[j],
                                  lambda_init, rope, need_ctx)
        xl = xl + mod_l[:, 2] * rms_norm(y_l, g[1])
        if need_ctx:
            xc = xc + mod_c[2] * rms_norm(y_c, g[1])

        h_l = modulate(xl, g[2], mod_l[:, 3], mod_l[:, 4])
        if need_ctx:
            h = jnp.concatenate([modulate(xc, g[2], mod_c[3], mod_c[4]), h_l], axis=1)
        else:
            h = h_l
        if i % 2 == 0:
            f = swiglu(h, ffn_w_gu[j], ffn_w_down[j])
        else:
            f = moe_ffn(h, moe_router[j], moe_w_gu[j], moe_w_down[j])
        xl = xl + mod_l[:, 5] * rms_norm(f[:, f.shape[1] - S:], g[3])
        if need_ctx:
            xc = xc + mod_c[5] * rms_norm(f[:, :C], g[3])
    return xl
```

```python
import math
import types
import numpy as np
import concourse.bass as bass
import concourse.mybir as mybir
from concourse.bass_utils import run_bass_kernel_spmd
from contextlib import ExitStack

F32 = mybir.dt.float32
BF16 = mybir.dt.bfloat16
AF = mybir.ActivationFunctionType
ALU = mybir.AluOpType
AX = mybir.AxisListType

ENGS = ("pe", "act", "dve", "pool", "sp")
EPS = 1e-6


class Tok:
    __slots__ = ("name", "w", "r")

    def __init__(self, name=""):
        self.name = name
        self.w = None
        self.r = []


class Buf:
    __slots__ = ("ap", "tok", "psum")

    def __init__(self, ap, name="", psum=False):
        self.ap = ap
        self.tok = Tok(name)
        self.psum = psum


class Op:
    __slots__ = ("eng", "fn", "waits", "flag", "idx", "dma_inc")

    def __init__(self, eng, fn, idx):
        self.eng = eng
        self.fn = fn
        self.waits = []
        self.flag = False
        self.idx = idx
        self.dma_inc = None


def _snap(fn):
    if fn is None or fn.__closure__ is None:
        return fn
    cells = []
    for c in fn.__closure__:
        try:
            cells.append(types.CellType(c.cell_contents))
        except ValueError:
            cells.append(c)
    return types.FunctionType(fn.__code__, fn.__globals__, fn.__name__, fn.__defaults__, tuple(cells))


class Prog:
    def __init__(self, nc):
        self.nc = nc
        self.ops = {e: [] for e in ENGS}
        self.wm = {e: {} for e in ENGS}
        self.dma_sems = {}
        self.es = ExitStack()

    def _need(self, op, ev):
        if ev is None:
            return
        if ev[0] == "e":
            _, eng, pop = ev
            if eng == "pe" and op.eng == "pe":
                return
            key = ("e", eng)
            cur = self.wm[op.eng].get(key, -1)
            if pop.idx <= cur:
                return
            self.wm[op.eng][key] = pop.idx
            pop.flag = True
            op.waits.append(ev)
        else:
            _, sname, val = ev
            key = ("d", sname)
            cur = self.wm[op.eng].get(key, -1)
            if val <= cur:
                return
            self.wm[op.eng][key] = val
            op.waits.append(ev)

    def _add(self, eng, fn, reads, writes):
        op = Op(eng, fn, len(self.ops[eng]))
        self.ops[eng].append(op)
        for t in reads:
            self._need(op, t.w)
        for t in writes:
            self._need(op, t.w)
            for ev in t.r:
                self._need(op, ev)
        return op

    def op(self, eng, fn, reads=(), writes=()):
        op = self._add(eng, _snap(fn), reads, writes)
        ev = ("e", eng, op)
        for t in reads:
            t.r = [x for x in t.r if not (x[0] == "e" and x[1] == eng)]
            t.r.append(ev)
        for t in writes:
            t.w = ev
            t.r = []
        return op

    def dma(self, q, sem, out, in_, reads=(), writes=(), **kw):
        def fn(e, out=out, in_=in_, kw=kw):
            return e.dma_start(out=out, in_=in_, **kw)
        saved = []
        for t in writes:
            if t.w is not None and t.w[0] == "d" and t.w[1] == sem:
                saved.append((t, t.w))
                t.w = None
        op = self._add(q, fn, reads, writes)
        for t, w in saved:
            t.w = w
        val = self.dma_sems.get(sem, 0) + 16
        self.dma_sems[sem] = val
        op.dma_inc = (sem, 16)
        ev = ("d", sem, val)
        for t in reads:
            t.r = [x for x in t.r if not (x[0] == "d" and x[1] == sem)]
            t.r.append(ev)
        for t in writes:
            t.w = ev
            t.r = []
        return op

    def coll(self, sem, groups, in_, out, reads=(), writes=()):
        def fn(e, in_=in_, out=out, groups=groups):
            return e.collective_compute("AllGather", ALU.bypass, replica_groups=groups,
                                        ins=[in_], outs=[out])
        if not hasattr(self, "cc_tok"):
            self.cc_tok = Tok("cc_serial")
        writes = list(writes) + [self.cc_tok]
        op = self._add("pool", fn, reads, writes)
        val = self.dma_sems.get(sem, 0) + 1
        self.dma_sems[sem] = val
        op.dma_inc = (sem, 1)
        ev = ("d", sem, val)
        for t in reads:
            t.r.append(ev)
        for t in writes:
            t.w = ev
            t.r = []
        return op

    def barrier(self):
        evs = []
        for e in ENGS:
            if e == "sp":
                continue
            for o in reversed(self.ops[e]):
                if o.fn is not None and o.dma_inc is None:
                    evs.append(("e", e, o))
                    break
        for s, v in self.dma_sems.items():
            evs.append(("d", s, v))
        bop = Op("sp", lambda e: e.nop(), len(self.ops["sp"]))
        self.ops["sp"].append(bop)
        for ev in evs:
            self._need(bop, ev)
        bev = ("e", "sp", bop)
        for e in ENGS:
            if e == "sp":
                continue
            o = Op(e, None, len(self.ops[e]))
            self.ops[e].append(o)
            self._need(o, bev)
        for e in ENGS:
            for x in ENGS:
                if self.ops[x]:
                    self.wm[e][("e", x)] = len(self.ops[x]) - 1
            for s_, v in self.dma_sems.items():
                self.wm[e][("d", s_)] = v

    def flush(self, final=False):
        nc = self.nc
        if not hasattr(self, "sem"):
            self.sem = {}
            self.emitted = {e: 0 for e in ENGS}
            self.cum = {e: 0 for e in ENGS}
            self.cnt = {}
        sem = self.sem
        for e in ENGS:
            if ("e", e) not in sem:
                sem[("e", e)] = self.es.enter_context(nc.semaphore("s_" + e))
        for s_ in self.dma_sems:
            if ("d", s_) not in sem:
                sem[("d", s_)] = self.es.enter_context(nc.semaphore("d_" + s_))
        cnt = self.cnt
        for e in ENGS:
            c = self.cum[e]
            for op in self.ops[e][self.emitted[e]:]:
                if op.flag:
                    c += 1
                cnt[(e, op.idx)] = c
            self.cum[e] = c
        ops = self.ops
        dma_sems = self.dma_sems
        start = dict(self.emitted)

        def emit(e, h):
            for op in ops[e][start[e]:]:
                for ev in op.waits:
                    if ev[0] == "e":
                        h.wait_ge(sem[("e", ev[1])], cnt[(ev[1], ev[2].idx)])
                    else:
                        h.wait_ge(sem[("d", ev[1])], ev[2])
                if op.fn is None:
                    continue
                ins = op.fn(h)
                if op.dma_inc is not None:
                    ins.then_inc(sem[("d", op.dma_inc[0])], op.dma_inc[1])
                elif op.flag:
                    ins.then_inc(sem[("e", e)], 1)
            if e == "sp" and final:
                for s_, v in dma_sems.items():
                    h.wait_ge(sem[("d", s_)], v)

        with nc.Block() as block:
            @block.tensor
            def _(h):
                emit("pe", h)

            @block.scalar
            def _(h):
                emit("act", h)

            @block.vector
            def _(h):
                emit("dve", h)

            @block.gpsimd
            def _(h):
                emit("pool", h)

            @block.sync
            def _(h):
                emit("sp", h)
        for e in ENGS:
            self.emitted[e] = len(self.ops[e])

    def finalize(self):
        self.flush(final=True)
        self.es.close()


class Arena:
    def __init__(self, nc, P):
        self.nc = nc
        self.P = P
        self.stacks = [P.es]
        self.n = 0
        self.used = [0]
        self.peak = 0

    def alloc(self, shape_free, dtype, parts=128, name="t"):
        shape_free = [int(s) for s in shape_free]
        self.n += 1
        t = self.stacks[-1].enter_context(self.nc.sbuf_tensor("%s_%d" % (name, self.n), [parts] + shape_free, dtype))
        self.used[-1] += int(np.prod(shape_free)) * (4 if dtype == F32 else 2)
        self.peak = max(self.peak, sum(self.used))
        return t[:]

    def buf(self, shape_free, dtype, name="", parts=128):
        return Buf(self.alloc(shape_free, dtype, parts, name or "t"), name)

    def mark(self):
        self.stacks.append(ExitStack())
        self.used.append(0)

    def release(self):
        self.P.barrier()
        self.P.flush()
        self.stacks.pop().close()
        self.used.pop()


class Ring:
    def __init__(self, bufs):
        self.bufs = bufs
        self.i = 0

    def next(self):
        b = self.bufs[self.i % len(self.bufs)]
        self.i += 1
        return b


def make_cfg(small=False):
    if small:
        c = dict(D=512, LT=512, CT=64, L0=256, LP=256, H=4, QR=256, KVR=128, H2=2, F=384, E=8, ED=128)
    else:
        c = dict(D=4096, LT=2048, CT=64, L0=256, LP=448, H=32, QR=1024, KVR=512, H2=16, F=3072, E=8, ED=1024)
    c["L"] = 4
    c["G"] = 4
    c["NC"] = 8
    c["B"] = 2
    c["T"] = c["CT"] + c["LT"]
    c["DC"] = c["D"] // 128
    c["GRID_W"] = 64
    c["S"] = c["LT"] * c["G"]
    c["C"] = c["CT"] * c["G"]
    assert c["H"] * 128 == c["D"] and c["H2"] * 256 == c["D"]
    assert (c["LT"] - c["L0"]) % c["LP"] == 0
    assert (6 * c["DC"]) % 8 == 0
    assert c["CT"] == 64 and c["T"] % 64 == 0
    c["CPR"] = 6 * c["DC"] // 8
    return c


def weight_list(cfg):
    D, QR, KVR, H, F, E, ED = cfg["D"], cfg["QR"], cfg["KVR"], cfg["H"], cfg["F"], cfg["E"], cfg["ED"]
    ws = []
    for j in range(2):
        ws += [("wdq%d" % j, D, QR), ("wuq%d" % j, QR, H * 192), ("wdkv%d" % j, D, KVR + 128),
               ("wukv%d" % j, KVR, H * 256), ("wom%d" % j, D, D),
               ("wgu%d" % j, D, 2 * F), ("wdn%d" % j, F, D)]
    for j in range(2):
        ws += [("wqkv%d" % j, D, 3 * D), ("wod%d" % j, D, D)]
        for e in range(E):
            ws += [("mgu%d_%d" % (j, e), D, 2 * ED), ("mdn%d_%d" % (j, e), ED, D)]
    return ws


import os as _os
CC_LIMIT = int(_os.environ.get("CC_LIMIT", 512 * 1024))


def colblock(Kd, N):
    lim = CC_LIMIT // ((Kd // 8) * 2)
    if N <= lim:
        return N
    best = 128
    for cw in range(128, lim + 1, 128):
        if N % cw == 0:
            best = cw
    return best


def passes_of(cfg):
    CT, L0, LP, LT = cfg["CT"], cfg["L0"], cfg["LP"], cfg["LT"]
    ps = [[(0, CT, 1), (CT, L0, 0)]]
    o = CT + L0
    while o < CT + LT:
        ps.append([(o, LP, 0)])
        o += LP
    return ps


def subtiles(tiles):
    out = []
    for (o, s, c) in tiles:
        a = 0
        while a < s:
            n = min(128, s - a)
            out.append((o + a, n))
            a += n
    return out


def rope_tables_np(cfg, r, dim):
    T, CT, LT, GW = cfg["T"], cfg["CT"], cfg["LT"], cfg["GRID_W"]
    half = dim // 2
    nfreq = half // 2
    inv = (10000.0 ** (-np.arange(0, half, 2, dtype=np.float32) / np.float32(half))).astype(np.float32)
    t = np.arange(r * LT, (r + 1) * LT)
    row = (t // GW).astype(np.float32)
    col = (t % GW).astype(np.float32)
    cos = np.ones((128, T), np.float32)
    sin = np.zeros((128, T), np.float32)
    for p in range(128):
        f = p % dim
        s = f // half
        g = f % half
        i = g % nfreq
        pos = row if s == 0 else col
        ang = (pos * inv[i]).astype(np.float32)
        cos[p, CT:] = np.cos(ang)
        sin[p, CT:] = np.sin(ang)
    return cos, sin


def perm_np(dim):
    half = dim // 2
    nfreq = half // 2
    m = np.zeros((128, 128), np.float32)
    for p in range(128):
        base = p - (p % dim)
        f = p % dim
        s = f // half
        g = f % half
        part = g // nfreq
        if part == 0:
            k = p + nfreq
            m[k, p] = -1.0
        else:
            k = p - nfreq
            m[k, p] = 1.0
    return m


def cols_np(v):
    return np.ascontiguousarray(v.reshape(-1, 128).T)


def prepare_inputs(cfg, inp):
    D, T, CT, LT, DC, L, NCR, G = cfg["D"], cfg["T"], cfg["CT"], cfg["LT"], cfg["DC"], cfg["L"], cfg["NC"], cfg["G"]
    H, QR, KVR, H2, F, E, ED, CPR = cfg["H"], cfg["QR"], cfg["KVR"], cfg["H2"], cfg["F"], cfg["E"], cfg["ED"], cfg["CPR"]
    f32 = np.float32
    x = np.asarray(inp["x"], f32)
    ctx = np.asarray(inp["ctx"], f32)
    cvec = np.stack([np.asarray(inp["c"], f32)[0], np.asarray(inp["c"], f32)[1], np.asarray(inp["c_ctx"], f32)], 0)
    cv = np.ascontiguousarray(cvec.reshape(3, DC, 128).transpose(2, 1, 0))
    ada_w = np.asarray(inp["ada_w"], f32)
    ada_b = np.asarray(inp["ada_b"], f32)
    ng = np.ascontiguousarray(np.asarray(inp["norm_g"], f32).reshape(L, 4, DC, 128).transpose(3, 0, 1, 2))
    full = {}
    for j in range(2):
        wuq = np.asarray(inp["mla_w_uq"], f32)[j].reshape(QR, H, 192)
        full["wdq%d" % j] = np.asarray(inp["mla_w_dq"], f32)[j]
        full["wuq%d" % j] = np.concatenate([wuq[:, :, :128].reshape(QR, H * 128), wuq[:, :, 128:].reshape(QR, H * 64)], 1)
        wdkv = np.asarray(inp["mla_w_dkv"], f32)[j]
        full["wdkv%d" % j] = np.concatenate([wdkv[:, :KVR], wdkv[:, KVR:], wdkv[:, KVR:]], 1)
        wukv = np.asarray(inp["mla_w_ukv"], f32)[j].reshape(KVR, H, 256)
        full["wukv%d" % j] = np.concatenate([wukv[:, :, :128].reshape(KVR, H * 128), wukv[:, :, 128:].reshape(KVR, H * 128)], 1)
        full["wom%d" % j] = np.asarray(inp["mla_w_o"], f32)[j]
        full["wgu%d" % j] = np.asarray(inp["ffn_w_gu"], f32)[j]
        full["wdn%d" % j] = np.asarray(inp["ffn_w_down"], f32)[j]
        full["wqkv%d" % j] = np.asarray(inp["diff_w_qkv"], f32)[j]
        full["wod%d" % j] = np.asarray(inp["diff_w_o"], f32)[j]
        for e in range(E):
            full["mgu%d_%d" % (j, e)] = np.asarray(inp["moe_w_gu"], f32)[j, e]
            full["mdn%d_%d" % (j, e)] = np.asarray(inp["moe_w_down"], f32)[j, e]
    pm = perm_np(64)
    pd = perm_np(128)
    maps = []
    for c in range(NCR):
        b, r = c // G, c % G
        sh = (c % 4) * 2 + c // 4
        m = {}
        xt = np.concatenate([ctx[b, r * CT:(r + 1) * CT], x[b, r * LT:(r + 1) * LT]], 0)
        m["xT"] = np.ascontiguousarray(xt.T)
        m["cv"] = cv
        m["adaw"] = np.ascontiguousarray(ada_w[:, :, sh * CPR * 128:(sh + 1) * CPR * 128])
        m["adab"] = np.ascontiguousarray(ada_b[:, sh * CPR * 128:(sh + 1) * CPR * 128].reshape(L, CPR, 128).transpose(2, 0, 1))
        sel = np.zeros((128, 3), f32)
        sel[:, b] = 1.0
        m["sel"] = sel
        m["ng"] = ng
        cm, sm = rope_tables_np(cfg, r, 64)
        cd, sd = rope_tables_np(cfg, r, 128)
        m["ropeM"] = np.ascontiguousarray(np.stack([cm, sm], 1))
        m["ropeD"] = np.ascontiguousarray(np.stack([cd, sd], 1))
        m["perms"] = np.ascontiguousarray(np.stack([pm, pd], 1))
        sm_ = np.zeros((E, E, 128), f32)
        for e_ in range(E):
            sm_[e_, e_, :] = 1.0
        m["selm"] = sm_.reshape(E, E * 128)
        for j in range(2):
            m["qn%d" % j] = cols_np(np.asarray(inp["mla_q_norm"], f32)[j])
            m["kvn%d" % j] = cols_np(np.asarray(inp["mla_kv_norm"], f32)[j])
            m["lam%d" % j] = np.ascontiguousarray(np.broadcast_to(np.asarray(inp["diff_lambda"], f32)[j][None], (128, 4, 128)))
            m["sln%d" % j] = cols_np(np.asarray(inp["diff_subln"], f32)[j])
            m["wr%d" % j] = np.ascontiguousarray(np.asarray(inp["moe_router"], f32)[j].reshape(DC, 128, E).transpose(1, 0, 2))
        for name, K, N in weight_list(cfg):
            m[name] = np.ascontiguousarray(full[name][sh * (K // 8):(sh + 1) * (K // 8)])
        maps.append(m)
    return maps


class K:
    pass


def build(cfg, nlayers=4, debug=(), stop=None):
    nc = bass.Bass("TRN2", target_bir_lowering=False)
    P = Prog(nc)
    es = P.es
    D, T, CT, LT, DC, L, G = cfg["D"], cfg["T"], cfg["CT"], cfg["LT"], cfg["DC"], cfg["L"], cfg["G"]
    H, QR, KVR, H2, F, E, ED, CPR = cfg["H"], cfg["QR"], cfg["KVR"], cfg["H2"], cfg["F"], cfg["E"], cfg["ED"], cfg["CPR"]
    QC, KVC = QR // 128, KVR // 128
    PASSES = passes_of(cfg)
    TPMAX = max(sum(s for (_, s, _) in p) for p in PASSES)
    WB = 256
    KCMAX = max(DC, F // 128, QC, KVC, ED // 128)

    def ext_in(name, shape, dt=F32):
        return nc.dram_tensor(name, list(shape), dt, kind="ExternalInput").ap()

    def dram(name, shape, dt):
        return nc.dram_tensor(name, list(shape), dt).ap()

    xT_in = ext_in("xT", [D, T])
    cv_in = ext_in("cv", [128, DC, 3])
    adaw_in = ext_in("adaw", [L, D, CPR * 128])
    adab_in = ext_in("adab", [128, L, CPR])
    sel_in = ext_in("sel", [128, 3])
    ng_in = ext_in("ng", [128, L, 4, DC])
    ropeM_in = ext_in("ropeM", [128, 2, T])
    ropeD_in = ext_in("ropeD", [128, 2, T])
    perms_in = ext_in("perms", [128, 2, 128])
    selm_in = ext_in("selm", [E, E * 128])
    small_in = {}
    for j in range(2):
        small_in["qn%d" % j] = ext_in("qn%d" % j, [128, QC])
        small_in["kvn%d" % j] = ext_in("kvn%d" % j, [128, KVC])
        small_in["lam%d" % j] = ext_in("lam%d" % j, [128, 4, 128])
        small_in["sln%d" % j] = ext_in("sln%d" % j, [128, 2])
        small_in["wr%d" % j] = ext_in("wr%d" % j, [128, DC, E])
    wl = weight_list(cfg)
    w_in, w_sh, w_full, w_tok, w_tmp, w_cw = {}, {}, {}, {}, {}, {}
    for name, Kd, N in wl:
        cw = colblock(Kd, N)
        ncb = N // cw
        w_cw[name] = (cw, ncb, Kd)
        w_in[name] = ext_in(name, [Kd // 8, N])
        w_sh[name] = dram(name + "_sh", [ncb * (Kd // 8), cw], BF16)
        w_full[name] = dram(name + "_bf", [ncb * Kd, cw], BF16)
        w_tmp[name] = dram(name + "_tmp", [ncb * (Kd // 4), cw], BF16)
        w_tok[name] = Tok(name)
    yT_out = nc.dram_tensor("yT", [D, T], F32, kind="ExternalOutput").ap()

    xcur = dram("xcur", [D, T], F32)
    modsh = dram("modsh", [128, L * CPR * 3], F32)
    modall = dram("modall", [8 * 128, L * CPR * 3], F32)
    modtmp = dram("modtmp", [2 * 128, L * CPR * 3], F32)
    NQC = max(H + H // 2, 2 * H2)
    NKC = max(H + 1, 2 * H2)
    QT = dram("QT", [NQC * 128, T], BF16)
    KTl = dram("KTl", [NKC * 128, T], BF16)
    KTg = dram("KTg", [G * NKC * 128, T], BF16)
    Vl = dram("Vl", [T, D], BF16)
    Vg = dram("Vg", [G * T, D], BF16)
    OT = dram("OT", [D, T], BF16)
    txc = [Tok("xc%d" % c) for c in range(DC)]
    dbg = {}
    for name, shape, dt in debug:
        dbg[name] = nc.dram_tensor("dbg_" + name, list(shape), dt, kind="ExternalOutput").ap()

    ar = Arena(nc, P)
    banks = [Buf(es.enter_context(nc.psum_tensor("pb%d" % i, [128, 512], F32))[:], "pb%d" % i, True) for i in range(8)]

    ones_f = ar.buf((128,), F32, "ones_f")
    ones_b = ar.buf((128,), BF16, "ones_b")
    perm_b = ar.buf((2, 128), BF16, "perm_b")
    modreg = ar.buf((2, L, 6 * DC), F32, "modreg")
    ngc = ar.buf((L, 4, DC), F32, "ngc")
    vecs = ar.buf((2, L, 6, DC), F32, "vecs")
    P.op("pool", lambda e: e.memset(ones_f.ap, 1.0), writes=[ones_f.tok])
    P.op("pool", lambda e: e.memset(ones_b.ap, 1.0), writes=[ones_b.tok])
    P.dma("pool", "c_perm", perm_b.ap, perms_in, writes=[perm_b.tok])
    P.dma("sp", "c_ng", ngc.ap, ng_in, writes=[ngc.tok])

    def ev(eng, fn, reads, writes):
        rd = [b.tok for b in reads if not b.psum]
        wr = [b.tok for b in writes] + [b.tok for b in reads if b.psum]
        return P.op(eng, fn, rd, wr)

    PAIRS = [[0, 4], [1, 5], [2, 6], [3, 7]]
    QUADS = [[0, 1, 2, 3], [4, 5, 6, 7]]

    def allgather8(src, tmp, dst, reads, tout):
        tt = Tok("ag_tmp")
        P.coll("cc", PAIRS, src.opt(), tmp.opt(), reads=reads, writes=[tt])
        P.coll("cc", QUADS, tmp.opt(), dst.opt(), reads=[tt], writes=[tout])

    def fin():
        P.flush(final=True)
        while len(ar.stacks) > 1:
            ar.stacks.pop().close()
        P.es.close()

    ar.mark()
    cvt = ar.buf((DC, 3), F32, "cvt")
    svt = ar.buf((DC, 3), F32, "svt")
    adab_t = ar.buf((L, CPR), F32, "adab")
    modloc = ar.buf((L, CPR, 3), F32, "modloc")
    selt = ar.buf((3,), F32, "sel")
    P.dma("sp", "c_cv", cvt.ap, cv_in, writes=[cvt.tok])
    P.dma("sp", "c_adab", adab_t.ap, adab_in, writes=[adab_t.tok])
    P.dma("sp", "c_sel", selt.ap, sel_in, writes=[selt.tok])
    ev("act", lambda e: e.activation(out=svt.ap, in_=cvt.ap, func=AF.Silu), [cvt], [svt])
    AWB = 512 if CPR * 128 >= 512 else CPR * 128
    nab = CPR * 128 // AWB
    KCH = 8 if DC >= 8 else DC
    awr = Ring([ar.buf((DC, AWB), F32, "aw%d" % i) for i in range(2)])
    adaw_v = adaw_in.rearrange("l (kc p) n -> l p kc n", p=128)
    for l in range(L):
        for ab in range(nab):
            wb = awr.next()
            for k0 in range(0, DC, KCH):
                P.dma("sp", "aw%d" % ((awr.i - 1) % 2), wb.ap[:, k0:k0 + KCH, :],
                      adaw_v[l, :, k0:k0 + KCH, ab * AWB:(ab + 1) * AWB], writes=[wb.tok])
            for mi in range(AWB // 128):
                ck = ab * (AWB // 128) + mi
                pb = banks[(l * nab * 4 + ab * 4 + mi) % 4]
                for kc in range(DC):
                    P.op("pe", (lambda e, o=pb.ap[:, 0:3], w=wb.ap[:, kc, mi * 128:(mi + 1) * 128], r=svt.ap[:, kc, :], s=(kc == 0), t=(kc == DC - 1):
                                e.matmul(o, w, r, start=s, stop=t)),
                         [wb.tok, svt.tok], [pb.tok])
                ev("dve", (lambda e, o=modloc.ap[:, l, ck, :], i=pb.ap[:, 0:3], b=adab_t.ap[:, l, ck:ck + 1]:
                           e.tensor_scalar(o, i, b, None, ALU.add)), [pb, adab_t], [modloc])
    if stop == "0a1":
        fin()
        return nc
    tmod = Tok("modsh")
    P.dma("pool", "s_mod", modsh, modloc.ap.rearrange("p l c j -> p (l c j)"), reads=[modloc.tok], writes=[tmod])
    tmodall = Tok("modall")
    allgather8(modsh, modtmp, modall, [tmod], tmodall)
    if stop == "0a2":
        fin()
        return nc
    modc = ar.buf((8, L, CPR, 3), F32, "modc")
    P.dma("pool", "c_modc", modc.ap.rearrange("p r l c j -> p r (l c j)"),
          modall.rearrange("(r p) f -> p r f", p=128), reads=[tmodall], writes=[modc.tok])
    if stop == "0a3":
        fin()
        return nc
    mr_lat = modreg.ap[:, 0].rearrange("p l (r c) -> p l r c", r=8)
    mr_ctx = modreg.ap[:, 1].rearrange("p l (r c) -> p l r c", r=8)
    for l in range(L):
        src = lambda j: modc.ap[:, :, l, :, j]
        ev("dve", (lambda e, o=mr_ctx[:, l], i=src(2): e.tensor_copy(o, i)), [modc], [modreg])
        ev("dve", (lambda e, o=mr_lat[:, l], i=src(0), s=selt.ap[:, 0:1]: e.tensor_scalar(o, i, s, None, ALU.mult)), [modc, selt], [modreg])
        ev("dve", (lambda e, o=mr_lat[:, l], i=src(1), s=selt.ap[:, 1:2]: e.scalar_tensor_tensor(o, i, s, o, ALU.mult, ALU.add)), [modc, selt, modreg], [modreg])
    if stop == "0a4":
        fin()
        return nc
    for cls in range(2):
        for l in range(L):
            md = lambda m: modreg.ap[:, cls, l, m * DC:(m + 1) * DC]
            g = lambda i: ngc.ap[:, l, i, :]
            v = lambda i: vecs.ap[:, cls, l, i, :]
            ev("dve", (lambda e, o=v(0), i=md(1), g_=g(0): e.scalar_tensor_tensor(o, i, 1.0, g_, ALU.add, ALU.mult)), [modreg, ngc], [vecs])
            ev("dve", (lambda e, o=v(1), i=md(0): e.tensor_copy(o, i)), [modreg], [vecs])
            ev("dve", (lambda e, o=v(2), i=md(2), g_=g(1): e.tensor_tensor(o, i, g_, ALU.mult)), [modreg, ngc], [vecs])
            ev("dve", (lambda e, o=v(3), i=md(4), g_=g(2): e.scalar_tensor_tensor(o, i, 1.0, g_, ALU.add, ALU.mult)), [modreg, ngc], [vecs])
            ev("dve", (lambda e, o=v(4), i=md(3): e.tensor_copy(o, i)), [modreg], [vecs])
            ev("dve", (lambda e, o=v(5), i=md(5), g_=g(3): e.tensor_tensor(o, i, g_, ALU.mult)), [modreg, ngc], [vecs])
    if "vecs" in dbg:
        P.dma("pool", "dbg", dbg["vecs"], vecs.ap.rearrange("p a l i c -> p (a l i c)"), reads=[vecs.tok])
    ar.release()
    if stop == "0a":
        fin()
        return nc

    used = set()
    for li in range(nlayers):
        j = li // 2
        if li % 2 == 0:
            used |= {"wdq%d" % j, "wuq%d" % j, "wdkv%d" % j, "wukv%d" % j, "wom%d" % j, "wgu%d" % j, "wdn%d" % j}
        else:
            used |= {"wqkv%d" % j, "wod%d" % j} | {"mgu%d_%d" % (j, e) for e in range(E)} | {"mdn%d_%d" % (j, e) for e in range(E)}
    for name, Kd, N in wl:
        if name not in used:
            continue
        rows = Kd // 8
        cw, ncb, _ = w_cw[name]
        for cb in range(ncb):
            tsh = Tok(name + "_sh")
            for r0 in range(0, rows, 128):
                r1 = min(rows, r0 + 128)
                P.dma("pool", "wcast", w_sh[name][cb * rows + r0:cb * rows + r1, :], w_in[name][r0:r1, cb * cw:(cb + 1) * cw], writes=[tsh])
            allgather8(w_sh[name][cb * rows:(cb + 1) * rows, :], w_tmp[name][cb * 2 * rows:(cb + 1) * 2 * rows, :],
                       w_full[name][cb * Kd:(cb + 1) * Kd, :], [tsh], w_tok[name])
    P.barrier()
    if stop == "0b":
        fin()
        return nc

    mmring = Ring(banks[0:4])
    B_SS, B_AUX, B_TM, B_RT = banks[4], banks[5], banks[6], banks[7]

    def vcol(cls, l, i, c):
        return vecs.ap[:, cls, l, i, c:c + 1]

    def load_weight_block(Wname, KC, ranges, wb, slot_sem):
        cw, ncb, Kd = w_cw[Wname]
        off = 0
        for (c0, n) in ranges:
            a = c0
            while a < c0 + n:
                cb = a // cw
                nn = min(c0 + n, (cb + 1) * cw) - a
                Wv = w_full[Wname][cb * Kd:(cb + 1) * Kd, :].rearrange("(kc p) n -> p kc n", p=128)
                for k0 in range(0, KC, 8):
                    k1 = min(KC, k0 + 8)
                    P.dma("sp", slot_sem, wb.ap[:, k0:k1, off:off + nn], Wv[:, k0:k1, a - cb * cw:a - cb * cw + nn],
                          reads=[w_tok[Wname]], writes=[wb.tok])
                off += nn
                a += nn

    def linear_fm(Wname, KC, blocks, in_ap, in_bufs, tiles, consumer, wring, local0=0):
        nb = len(blocks)
        wbs = [None] * nb

        def load(bi):
            wb = wring.next()
            wbs[bi] = wb
            load_weight_block(Wname, KC, blocks[bi], wb, "w%d" % ((wring.i - 1) % len(wring.bufs)))
        load(0)
        for bi in range(nb):
            if bi + 1 < nb:
                load(bi + 1)
            wb = wbs[bi]
            nm = sum(n for (_, n) in blocks[bi]) // 128
            for mi in range(nm):
                for (o, s, cls) in tiles:
                    pb = mmring.next()
                    for kc in range(KC):
                        P.op("pe", (lambda e, out=pb.ap[:, 0:s], w=wb.ap[:, kc, mi * 128:(mi + 1) * 128], r=in_ap(kc, o - local0, s), st=(kc == 0), sp=(kc == KC - 1):
                                    e.matmul(out, w, r, start=st, stop=sp)),
                             [wb.tok] + [b.tok for b in in_bufs], [pb.tok])
                    consumer(bi, mi, (o, s, cls), pb)

    def linear_tm(Wname, KC, blocks, in_ap, in_bufs, stiles, consumer, wring, local0=0):
        nb = len(blocks)
        wbs = [None] * nb

        def load(bi):
            wb = wring.next()
            wbs[bi] = wb
            load_weight_block(Wname, KC, blocks[bi], wb, "w%d" % ((wring.i - 1) % len(wring.bufs)))
        load(0)
        for bi in range(nb):
            if bi + 1 < nb:
                load(bi + 1)
            wb = wbs[bi]
            ncol = sum(n for (_, n) in blocks[bi])
            for (o, s) in stiles:
                pb = mmring.next()
                for kc in range(KC):
                    P.op("pe", (lambda e, out=pb.ap[0:s, 0:ncol], a=in_ap(kc, o - local0, s), w=wb.ap[:, kc, 0:ncol], st=(kc == 0), sp=(kc == KC - 1):
                                e.matmul(out, a, w, start=st, stop=sp)),
                         [wb.tok] + [b.tok for b in in_bufs], [pb.tok])
                consumer(bi, (o, s), pb, ncol)

    def rstd_from_bank(bank, n, dim, out):
        ev("dve", (lambda e, o=out.ap[:, 0:n], i=bank.ap[:, 0:n]: e.tensor_scalar(o, i, 1.0 / dim, EPS, ALU.mult, ALU.add)), [bank], [out])
        ev("act", (lambda e, o=out.ap[:, 0:n]: e.activation(out=o, in_=o, func=AF.Sqrt)), [out], [out])
        ev("dve", (lambda e, o=out.ap[:, 0:n]: e.reciprocal(o, o)), [out], [out])

    def ss_accum(src_ap, src_bufs, n, col0, first, last, sqring):
        sq = sqring.next()
        sq_ap = sq.ap
        ev("act", (lambda e, o=sq_ap[:, 0:n], i=src_ap: e.activation(out=o, in_=i, func=AF.Square)), src_bufs, [sq])
        P.op("pe", (lambda e, o=B_SS.ap[:, col0:col0 + n], r=sq_ap[:, 0:n], st=(first and col0 == 0), sp=last: e.matmul(o, ones_f.ap, r, start=st, stop=sp, skip_group_check=True)),
             [sq.tok, ones_f.tok], [B_SS.tok])

    def prenorm(xsrc, tiles, TP, p0, l, ai, bi_, hT, xring, sqring, rstd, tmpring, extra=None):
        for c in range(DC):
            xb = xring.next()
            P.dma("sp", "x%d" % ((xring.i - 1) % len(xring.bufs)), xb.ap[:, 0:TP], xsrc[c * 128:(c + 1) * 128, p0:p0 + TP], reads=[txc[c]], writes=[xb.tok])
            ss_accum(xb.ap[:, 0:TP], [xb], TP, 0, c == 0, c == DC - 1, sqring)
        rstd_from_bank(B_SS, TP, D, rstd)
        for c in range(DC):
            xb = xring.next()
            P.dma("sp", "x%d" % ((xring.i - 1) % len(xring.bufs)), xb.ap[:, 0:TP], xsrc[c * 128:(c + 1) * 128, p0:p0 + TP], reads=[txc[c]], writes=[xb.tok])
            tb = tmpring.next()
            for (o, s, cls) in tiles:
                lo = o - p0
                ev("dve", (lambda e, out=tb.ap[:, lo:lo + s], x=xb.ap[:, lo:lo + s], a=vcol(cls, l, ai, c), r=rstd.ap[:, lo:lo + s]:
                           e.scalar_tensor_tensor(out, x, a, r, ALU.mult, ALU.mult)), [xb, vecs, rstd], [tb])
                ev("pool", (lambda e, out=tb.ap[:, lo:lo + s], b=vcol(cls, l, bi_, c): e.tensor_scalar(out, out, b, None, ALU.add)), [tb, vecs], [tb])
            if extra is not None:
                extra(c, tb)
            ev("act", (lambda e, out=hT.ap[:, c, 0:TP], i=tb.ap[:, 0:TP]: e.activation(out=out, in_=i, func=AF.Copy)), [tb], [hT])

    stg_cnt = [0]

    def store(dst, src_buf, src_ap, writes=()):
        P.dma("pool", "s_" + src_buf.tok.name, dst, src_ap, reads=[src_buf.tok], writes=list(writes))

    def rope_store(pb, o, s, p0, rtab, pidx, premul, stg_ring, tmpring, dst):
        lo = o - p0
        qraw = stg_ring.next()
        if premul is not None:
            ev("dve", (lambda e, out=qraw.ap[:, 0:s], i=pb.ap[:, 0:s], r=premul: e.tensor_tensor(out, i, r, ALU.mult)), [pb], [qraw])
        else:
            ev("act", (lambda e, out=qraw.ap[:, 0:s], i=pb.ap[:, 0:s]: e.activation(out=out, in_=i, func=AF.Copy)), [pb], [qraw])
        P.op("pe", (lambda e, out=B_AUX.ap[:, 0:s], r=qraw.ap[:, 0:s]: e.matmul(out, perm_b.ap[:, pidx, :], r, start=True, stop=True)),
             [qraw.tok, perm_b.tok], [B_AUX.tok])
        t1 = tmpring.next()
        t2 = tmpring.next()
        ev("dve", (lambda e, out=t1.ap[:, 0:s], i=qraw.ap[:, 0:s], c_=rtab.ap[:, 0, lo:lo + s]: e.tensor_tensor(out, i, c_, ALU.mult)), [qraw, rtab], [t1])
        ev("dve", (lambda e, out=t2.ap[:, 0:s], i=B_AUX.ap[:, 0:s], s_=rtab.ap[:, 1, lo:lo + s]: e.tensor_tensor(out, i, s_, ALU.mult)), [B_AUX, rtab], [t2])
        qo = stg_ring.next()
        ev("pool", (lambda e, out=qo.ap[:, 0:s], a=t1.ap[:, 0:s], b=t2.ap[:, 0:s]: e.tensor_tensor(out, a, b, ALU.add)), [t1, t2], [qo])
        store(dst, qo, qo.ap[:, 0:s])

    def phase_A(l, xsrc):
        mla = (l % 2 == 0)
        j = l // 2
        ar.mark()
        hT = ar.buf((DC, TPMAX), BF16, "hT")
        wring = Ring([ar.buf((KCMAX, WB), BF16, "wb%d" % i) for i in range(2)])
        xring = Ring([ar.buf((TPMAX,), F32, "xb%d" % i) for i in range(3)])
        sqring = Ring([ar.buf((TPMAX,), F32, "sq%d" % i) for i in range(2)])
        tmpring = Ring([ar.buf((TPMAX,), F32, "tmp%d" % i) for i in range(4)])
        stg = Ring([ar.buf((512,), BF16, "stg%d" % i) for i in range(6)])
        rstd = ar.buf((TPMAX,), F32, "rstd")
        rtab = ar.buf((2, TPMAX), F32, "rtab")
        if mla:
            qaT = ar.buf((QC, TPMAX), BF16, "qaT")
            ckT = ar.buf((KVC, TPMAX), BF16, "ckT")
            cksq = ar.buf((KVC, TPMAX), F32, "cksq")
            rq = ar.buf((TPMAX,), F32, "rq")
            rkv = ar.buf((TPMAX,), F32, "rkv")
            rkvc = ar.buf((8,), F32, "rkvc")
            qn = ar.buf((QC,), F32, "qn")
            kvn = ar.buf((KVC,), F32, "kvn")
            P.dma("sp", "c_qn", qn.ap, small_in["qn%d" % j], writes=[qn.tok])
            P.dma("sp", "c_kvn", kvn.ap, small_in["kvn%d" % j], writes=[kvn.tok])
        for tiles in PASSES:
            p0 = tiles[0][0]
            TP = sum(s for (_, s, _) in tiles)
            P.dma("sp", "c_rt", rtab.ap[:, :, 0:TP], (ropeM_in if mla else ropeD_in)[:, :, p0:p0 + TP], writes=[rtab.tok])
            prenorm(xsrc, tiles, TP, p0, l, 0, 1, hT, xring, sqring, rstd, tmpring)
            h_in = lambda kc, o, s: hT.ap[:, kc, o:o + s]
            if mla:
                def c_qa(bi, mi, tile, pb):
                    o, s, cls = tile
                    m = bi * (WB // 128) + mi
                    lo = o - p0
                    ss_accum(pb.ap[:, 0:s], [pb], s, lo, m == 0, m == QC - 1, sqring)
                    ev("dve", (lambda e, out=qaT.ap[:, m, lo:lo + s], i=pb.ap[:, 0:s], g=qn.ap[:, m:m + 1]: e.tensor_scalar(out, i, g, None, ALU.mult)), [pb, qn], [qaT])
                blocks = [[(c0, min(WB, QR - c0))] for c0 in range(0, QR, WB)]
                linear_fm("wdq%d" % j, DC, blocks, h_in, [hT], tiles, c_qa, wring, p0)
                rstd_from_bank(B_SS, TP, QR, rq)
                qa_in = lambda kc, o, s: qaT.ap[:, kc, o:o + s]

                def c_q(bi, mi, tile, pb):
                    o, s, cls = tile
                    m = bi * (WB // 128) + mi
                    lo = o - p0
                    if m < H:
                        sb = stg.next()
                        ev("dve", (lambda e, out=sb.ap[:, 0:s], i=pb.ap[:, 0:s], r=rq.ap[:, lo:lo + s]: e.tensor_tensor(out, i, r, ALU.mult)), [pb, rq], [sb])
                        store(QT[m * 128:(m + 1) * 128, o:o + s], sb, sb.ap[:, 0:s])
                    else:
                        rope_store(pb, o, s, p0, rtab, 0, rq.ap[:, lo:lo + s], stg, tmpring, QT[m * 128:(m + 1) * 128, o:o + s])
                NQ = H * 192
                blocks = [[(c0, min(WB, NQ - c0))] for c0 in range(0, NQ, WB)]
                linear_fm("wuq%d" % j, QC, blocks, qa_in, [qaT, rq], tiles, c_q, wring, p0)
                def c_kv(bi, mi, tile, pb):
                    o, s, cls = tile
                    m = bi * (WB // 128) + mi
                    lo = o - p0
                    if m < KVC:
                        ev("act", (lambda e, out=cksq.ap[:, m, lo:lo + s], i=pb.ap[:, 0:s]: e.activation(out=out, in_=i, func=AF.Square)), [pb], [cksq])
                        P.op("pe", (lambda e, out=B_SS.ap[:, lo:lo + s], r=cksq.ap[:, m, lo:lo + s], st=(m == 0 and lo == 0), sp=(m == KVC - 1): e.matmul(out, ones_f.ap, r, start=st, stop=sp, skip_group_check=True)),
                             [cksq.tok, ones_f.tok], [B_SS.tok])
                        ev("dve", (lambda e, out=ckT.ap[:, m, lo:lo + s], i=pb.ap[:, 0:s], g=kvn.ap[:, m:m + 1]: e.tensor_scalar(out, i, g, None, ALU.mult)), [pb, kvn], [ckT])
                    else:
                        rope_store(pb, o, s, p0, rtab, 0, None, stg, tmpring, KTl[H * 128:(H + 1) * 128, o:o + s])
                NKV = KVR + 128
                blocks = [[(c0, min(WB, NKV - c0))] for c0 in range(0, NKV, WB)]
                linear_fm("wdkv%d" % j, DC, blocks, h_in, [hT], tiles, c_kv, wring, p0)
                rstd_from_bank(B_SS, TP, KVR, rkv)
                sts = subtiles(tiles)
                for si, (o, s) in enumerate(sts):
                    lo = o - p0
                    for m in range(KVC):
                        P.op("pe", (lambda e, out=B_TM.ap[0:s, si:si + 1], a=cksq.ap[:, m, lo:lo + s], st=(m == 0), sp=(m == KVC - 1): e.matmul(out, a, ones_f.ap[:, 0:1], start=st, stop=sp)),
                             [cksq.tok, ones_f.tok], [B_TM.tok])
                nst = len(sts)
                ev("dve", (lambda e, o_=rkvc.ap[:, 0:nst], i=B_TM.ap[:, 0:nst]: e.tensor_scalar(o_, i, 1.0 / KVR, EPS, ALU.mult, ALU.add)), [B_TM], [rkvc])
                ev("act", (lambda e, o_=rkvc.ap[:, 0:nst]: e.activation(out=o_, in_=o_, func=AF.Sqrt)), [rkvc], [rkvc])
                ev("dve", (lambda e, o_=rkvc.ap[:, 0:nst]: e.reciprocal(o_, o_)), [rkvc], [rkvc])
                ck_in = lambda kc, o, s: ckT.ap[:, kc, o:o + s]

                def c_k(bi, mi, tile, pb):
                    o, s, cls = tile
                    m = bi * (WB // 128) + mi
                    lo = o - p0
                    sb = stg.next()
                    ev("dve", (lambda e, out=sb.ap[:, 0:s], i=pb.ap[:, 0:s], r=rkv.ap[:, lo:lo + s]: e.tensor_tensor(out, i, r, ALU.mult)), [pb, rkv], [sb])
                    store(KTl[m * 128:(m + 1) * 128, o:o + s], sb, sb.ap[:, 0:s])
                blocks = [[(c0, WB)] for c0 in range(0, H * 128, WB)]
                linear_fm("wukv%d" % j, KVC, blocks, ck_in, [ckT, rkv], tiles, c_k, wring, p0)
                def c_v(bi, st_, pb, ncol):
                    o, s = st_
                    si = sts.index(st_)
                    sb = stg.next()
                    ev("dve", (lambda e, out=sb.ap[0:s, 0:ncol], i=pb.ap[0:s, 0:ncol], r=rkvc.ap[0:s, si:si + 1]: e.tensor_scalar(out, i, r, None, ALU.mult)), [pb, rkvc], [sb])
                    store(Vl[o:o + s, bi * WB:bi * WB + ncol], sb, sb.ap[0:s, 0:ncol])
                blocks = [[(H * 128 + c0, WB)] for c0 in range(0, H * 128, WB)]
                linear_tm("wukv%d" % j, KVC, blocks, ck_in, [ckT, rkvc], sts, c_v, wring, p0)
            else:
                def c_qk(base):
                    def f(bi, mi, tile, pb):
                        o, s, cls = tile
                        m = bi * (WB // 128) + mi
                        dst = (QT if base == 0 else KTl)[m * 128:(m + 1) * 128, o:o + s]
                        rope_store(pb, o, s, p0, rtab, 1, None, stg, tmpring, dst)
                    return f
                blocks = [[(c0, WB)] for c0 in range(0, D, WB)]
                linear_fm("wqkv%d" % j, DC, blocks, h_in, [hT], tiles, c_qk(0), wring, p0)
                blocks = [[(D + c0, WB)] for c0 in range(0, D, WB)]
                linear_fm("wqkv%d" % j, DC, blocks, h_in, [hT], tiles, c_qk(1), wring, p0)
                sts = subtiles(tiles)

                def c_v(bi, st_, pb, ncol):
                    o, s = st_
                    sb = stg.next()
                    ev("act", (lambda e, out=sb.ap[0:s, 0:ncol], i=pb.ap[0:s, 0:ncol]: e.activation(out=out, in_=i, func=AF.Copy)), [pb], [sb])
                    store(Vl[o:o + s, bi * WB:bi * WB + ncol], sb, sb.ap[0:s, 0:ncol])
                blocks = [[(2 * D + c0, WB)] for c0 in range(0, D, WB)]
                linear_tm("wqkv%d" % j, DC, blocks, h_in, [hT], sts, c_v, wring, p0)
        ar.release()
        P.barrier()
        tk, tv = Tok("ktg"), Tok("vg")
        grp = QUADS
        nkc = (H + 1) if mla else 2 * H2
        for kc in range(nkc):
            P.coll("cc", grp, KTl[kc * 128:(kc + 1) * 128, :].opt(), KTg[kc * G * 128:(kc + 1) * G * 128, :].opt(), writes=[tk])
        for jv in range(T // 64):
            P.coll("cc", grp, Vl[jv * 64:(jv + 1) * 64, :].opt(), Vg[jv * G * 64:(jv + 1) * G * 64, :].opt(), writes=[tv])
        P.barrier()
        return nkc

    def phase_B(l, nkc):
        mla = (l % 2 == 0)
        j = l // 2
        ar.mark()
        scale = (192.0 ** -0.5) if mla else (128.0 ** -0.5)
        li = 0.8 - 0.6 * math.exp(-0.3 * l)
        KTv = [KTg[kc_ * G * 128:(kc_ + 1) * G * 128, :].rearrange("(g p) t -> p g t", p=128) for kc_ in range(nkc)]
        Vv = Vg.rearrange("(j g q) d -> q j g d", g=G, q=64)
        ktr = Ring([ar.buf((G, T), BF16, "kt%d" % i) for i in range(2 if mla else 4)])
        ndv = 1 if mla else 2
        NKT = 1 + LT // 128
        vtr = Ring([ar.buf((G, NKT, ndv * 128), BF16, "vt%d" % i) for i in range(2)])
        qtr = Ring([ar.buf((T,), BF16, "qt%d" % i) for i in range(4)])
        ptr = Ring([ar.buf((512,), BF16, "pT%d" % i) for i in range(6)])
        rlr = Ring([ar.buf((512,), F32, "rl%d" % i) for i in range(2)])
        accr = Ring([ar.buf((512,), F32, "acc%d" % i) for i in range(2)])
        ostg = Ring([ar.buf((512,), BF16, "os%d" % i) for i in range(4)])
        of = Ring([ar.buf((2, 512), F32, "of%d" % i) for i in range(2)])
        sq2 = Ring([ar.buf((512,), F32, "sq2_%d" % i) for i in range(2)])
        rs2 = ar.buf((512,), F32, "rs2")
        if mla:
            sring, oring, lring = Ring(banks[0:4]), Ring(banks[4:6]), Ring(banks[6:8])
        else:
            sring, oring, lring = Ring(banks[0:3]), Ring(banks[3:7]), Ring(banks[7:8])
        if mla:
            krope = ar.buf((G, T), BF16, "krope")
            P.dma("sp", "c_kr", krope.ap, KTv[H], writes=[krope.tok])
        else:
            lamt = ar.buf((4, 128), F32, "lamt")
            lt2 = ar.buf((2, 128), F32, "lt2")
            lsum = ar.buf((4,), F32, "lsum")
            sln = ar.buf((2,), F32, "sln")
            P.dma("sp", "c_lam", lamt.ap, small_in["lam%d" % j], writes=[lamt.tok])
            P.dma("sp", "c_sln", sln.ap, small_in["sln%d" % j], writes=[sln.tok])
            ev("dve", lambda e: e.tensor_tensor(lt2.ap[:, 0, :], lamt.ap[:, 0, :], lamt.ap[:, 1, :], ALU.mult), [lamt], [lt2])
            ev("dve", lambda e: e.tensor_tensor(lt2.ap[:, 1, :], lamt.ap[:, 2, :], lamt.ap[:, 3, :], ALU.mult), [lamt], [lt2])
            ev("dve", lambda e: e.reduce_sum(lsum.ap[:, 0:2], lt2.ap, AX.X), [lt2], [lsum])
            ev("act", lambda e: e.activation(out=lsum.ap[:, 0:2], in_=lsum.ap[:, 0:2], func=AF.Exp), [lsum], [lsum])
            ev("dve", lambda e: e.tensor_tensor(lsum.ap[:, 2:3], lsum.ap[:, 0:1], lsum.ap[:, 1:2], ALU.subtract), [lsum], [lsum])
            ev("dve", lambda e: e.tensor_scalar(lsum.ap[:, 2:3], lsum.ap[:, 2:3], float(li), None, ALU.add), [lsum], [lsum])
            ev("dve", lambda e: e.tensor_scalar(sln.ap, sln.ap, float(1.0 - li), None, ALU.mult), [sln], [sln])
        ktiles_all = []
        for g in range(G):
            ktiles_all.append((g, 0, 0, CT))
            for i in range(LT // 128):
                ktiles_all.append((g, 1 + i, CT + i * 128, 128))
        ktiles_ctx = [(g, 0, 0, CT) for g in range(G)]
        qblocks = [(0, CT, ktiles_ctx)]
        QB = 512
        for o in range(CT, T, QB):
            qblocks.append((o, min(QB, T - o), ktiles_all))
        nheads = H if mla else H2
        for h in range(nheads):
            vt = vtr.next()
            vs = "v%d" % ((vtr.i - 1) % 2)
            c0 = h * ndv * 128
            for g in range(G):
                P.dma("sp", vs, vt.ap[0:CT, g, 0, :], Vv[:, 0, g, c0:c0 + ndv * 128], writes=[vt.tok])
                for a_ in range(2):
                    P.dma("sp", vs, vt.ap[a_ * 64:(a_ + 1) * 64, g, 1:NKT, :], Vv[:, 1 + a_::2, g, c0:c0 + ndv * 128], writes=[vt.tok])
            maps = []
            for n in range(1 if mla else 2):
                kc = h if mla else 2 * h + n
                kt = ktr.next()
                P.dma("sp", "k%d" % ((ktr.i - 1) % len(ktr.bufs)), kt.ap, KTv[kc], writes=[kt.tok])
                qt = qtr.next()
                P.dma("sp", "q%d" % ((qtr.i - 1) % 4), qt.ap, QT[kc * 128:(kc + 1) * 128, :], writes=[qt.tok])
                parts = [(kt, qt, 0, 128)]
                if mla:
                    qr = qtr.next()
                    rc = H + h // 2
                    P.dma("sp", "q%d" % ((qtr.i - 1) % 4), qr.ap, QT[rc * 128:(rc + 1) * 128, :], writes=[qr.tok])
                    plo = (h % 2) * 64
                    parts.append((krope, qr, plo, plo + 64))
                maps.append(parts)
            for (qo, qs, ktl) in qblocks:
                res = []
                for n, parts in enumerate(maps):
                    obs = [oring.next() for _ in range(ndv)]
                    lb = lring.next()
                    nk = len(ktl)
                    acc = accr.next()
                    ev("pool", (lambda e, out=acc.ap[:, 0:qs]: e.memset(out, 0.0)), [], [acc])
                    LOOK = 2
                    pend = []
                    for kstep in range(nk + LOOK):
                        if kstep < nk:
                            (g, vi, ko, ks) = ktl[kstep]
                            sb_ = sring.next()
                            for pi, (kt, qt, plo, phi) in enumerate(parts):
                                P.op("pe", (lambda e, out=sb_.ap[0:ks, 0:qs], k_=kt.ap[plo:phi, g, ko:ko + ks], q_=qt.ap[plo:phi, qo:qo + qs], st=(pi == 0), sp=(pi == len(parts) - 1):
                                            e.matmul(out, k_, q_, start=st, stop=sp)),
                                     [kt.tok, qt.tok], [sb_.tok])
                            pT = ptr.next()
                            ev("act", (lambda e, out=pT.ap[0:ks, 0:qs], i=sb_.ap[0:ks, 0:qs]: e.activation(out=out, in_=i, func=AF.Exp, scale=float(scale))), [sb_], [pT])
                            ev("dve", (lambda e, a_=acc.ap[0:ks, 0:qs], p_=pT.ap[0:ks, 0:qs]: e.tensor_tensor(a_, a_, p_, ALU.add)), [acc, pT], [acc])
                            pend.append((kstep, g, vi, ks, pT))
                        if kstep >= LOOK:
                            (ki, g, vi, ks, pT) = pend.pop(0)
                            for dv in range(ndv):
                                P.op("pe", (lambda e, out=obs[dv].ap[:, 0:qs], v_=vt.ap[0:ks, g, vi, dv * 128:(dv + 1) * 128], p_=pT.ap[0:ks, 0:qs], st=(ki == 0), sp=(ki == nk - 1):
                                            e.matmul(out, v_, p_, start=st, stop=sp)),
                                     [vt.tok, pT.tok], [obs[dv].tok])
                    P.op("pe", (lambda e, out=lb.ap[:, 0:qs], a_=acc.ap[:, 0:qs]: e.matmul(out, ones_f.ap, a_, start=True, stop=True)),
                         [ones_f.tok, acc.tok], [lb.tok])
                    rl = rlr.next()
                    ev("dve", (lambda e, out=rl.ap[:, 0:qs], i=lb.ap[:, 0:qs]: e.reciprocal(out, i)), [lb], [rl])
                    res.append((obs, rl))
                if mla:
                    obs, rl = res[0]
                    sb = ostg.next()
                    ev("dve", (lambda e, out=sb.ap[:, 0:qs], i=obs[0].ap[:, 0:qs], r=rl.ap[:, 0:qs]: e.tensor_tensor(out, i, r, ALU.mult)), [obs[0], rl], [sb])
                    store(OT[h * 128:(h + 1) * 128, qo:qo + qs], sb, sb.ap[:, 0:qs])
                else:
                    (ob1, rl1), (ob2, rl2) = res
                    ev("dve", (lambda e, out=rl2.ap[:, 0:qs], lam=lsum.ap[:, 2:3]: e.tensor_scalar(out, out, lam, None, ALU.mult)), [rl2, lsum], [rl2])
                    ofb = of.next()
                    for dv in range(2):
                        sq = sq2.next()
                        ev("dve", (lambda e, out=ofb.ap[:, dv, 0:qs], i=ob1[dv].ap[:, 0:qs], r=rl1.ap[:, 0:qs]: e.tensor_tensor(out, i, r, ALU.mult)), [ob1[dv], rl1], [ofb])
                        ev("dve", (lambda e, out=sq.ap[:, 0:qs], i=ob2[dv].ap[:, 0:qs], r=rl2.ap[:, 0:qs]: e.tensor_tensor(out, i, r, ALU.mult)), [ob2[dv], rl2], [sq])
                        ev("pool", (lambda e, out=ofb.ap[:, dv, 0:qs], b=sq.ap[:, 0:qs]: e.tensor_tensor(out, out, b, ALU.subtract)), [ofb, sq], [ofb])
                        ev("act", (lambda e, out=sq.ap[:, 0:qs], i=ofb.ap[:, dv, 0:qs]: e.activation(out=out, in_=i, func=AF.Square)), [ofb], [sq])
                        ssb = ob1[0]
                        P.op("pe", (lambda e, out=ssb.ap[:, 0:qs], r=sq.ap[:, 0:qs], st=(dv == 0), sp=(dv == 1): e.matmul(out, ones_f.ap, r, start=st, stop=sp)),
                             [sq.tok, ones_f.tok], [ssb.tok])
                    ev("dve", (lambda e, out=rs2.ap[:, 0:qs], i=ob1[0].ap[:, 0:qs]: e.tensor_scalar(out, i, 1.0 / 256, EPS, ALU.mult, ALU.add)), [ob1[0]], [rs2])
                    ev("act", (lambda e, out=rs2.ap[:, 0:qs]: e.activation(out=out, in_=out, func=AF.Sqrt)), [rs2], [rs2])
                    ev("dve", (lambda e, out=rs2.ap[:, 0:qs]: e.reciprocal(out, out)), [rs2], [rs2])
                    for dv in range(2):
                        sb = ostg.next()
                        ev("dve", (lambda e, out=sb.ap[:, 0:qs], i=ofb.ap[:, dv, 0:qs], g_=sln.ap[:, dv:dv + 1], r=rs2.ap[:, 0:qs]:
                                   e.scalar_tensor_tensor(out, i, g_, r, ALU.mult, ALU.mult)), [ofb, sln, rs2], [sb])
                        store(OT[(2 * h + dv) * 128:(2 * h + dv + 1) * 128, qo:qo + qs], sb, sb.ap[:, 0:qs])
        ar.release()
        P.barrier()

    def phase_C(l, xsrc, xdst_final, last_layer):
        mla = (l % 2 == 0)
        j = l // 2
        moe = not mla
        ar.mark()
        oT = ar.buf((DC, TPMAX), BF16, "oT")
        yT = ar.buf((DC, TPMAX), F32, "yT")
        EDl = ED if moe else F
        EC = EDl // 128
        aT = ar.buf((EC, TPMAX), BF16, "aT")
        wring = Ring([ar.buf((KCMAX, WB), BF16, "wb%d" % i) for i in range(2)])
        xring = Ring([ar.buf((TPMAX,), F32, "xb%d" % i) for i in range(3)])
        sqring = Ring([ar.buf((TPMAX,), F32, "sq%d" % i) for i in range(2)])
        tmpring = Ring([ar.buf((TPMAX,), F32, "tmp%d" % i) for i in range(3)])
        sgr = Ring([ar.buf((TPMAX,), F32, "sg%d" % i) for i in range(2)])
        rstd = ar.buf((TPMAX,), F32, "rstd")
        if moe:
            comb = ar.buf((E, TPMAX), F32, "comb")
            lg = ar.buf((E, TPMAX), F32, "lg")
            lgs = ar.buf((TPMAX,), F32, "lgs", parts=E)
            m1 = ar.buf((TPMAX,), F32, "m1")
            m2 = ar.buf((TPMAX,), F32, "m2")
            w1 = ar.buf((TPMAX,), F32, "w1")
            wr = ar.buf((DC, E), F32, "wr")
            selm = ar.buf((E, 128), F32, "selm", parts=E)
            P.dma("sp", "c_wr", wr.ap, small_in["wr%d" % j], writes=[wr.tok])
            P.dma("sp", "c_selm", selm.ap.rearrange("p e q -> p (e q)"), selm_in, writes=[selm.tok])
        for tiles in PASSES:
            p0 = tiles[0][0]
            TP = sum(s for (_, s, _) in tiles)
            for c0 in range(0, DC, 8):
                c1 = min(DC, c0 + 8)
                P.dma("sp", "c_ot", oT.ap[:, c0:c1, 0:TP], OT[c0 * 128:c1 * 128, p0:p0 + TP].rearrange("(c p) t -> p c t", p=128), writes=[oT.tok])
            o_in = lambda kc, o, s: oT.ap[:, kc, o:o + s]

            def c_y(bi, mi, tile, pb):
                o, s, cls = tile
                m = bi * (WB // 128) + mi
                lo = o - p0
                ev("dve", (lambda e, out=yT.ap[:, m, lo:lo + s], i=pb.ap[:, 0:s]: e.tensor_copy(out, i)), [pb], [yT])
                sq = sqring.next()
                ev("act", (lambda e, out=sq.ap[:, 0:s], i=pb.ap[:, 0:s]: e.activation(out=out, in_=i, func=AF.Square)), [pb], [sq])
                P.op("pe", (lambda e, out=B_SS.ap[:, lo:lo + s], r=sq.ap[:, 0:s], st=(m == 0 and lo == 0), sp=(m == DC - 1): e.matmul(out, ones_f.ap, r, start=st, stop=sp, skip_group_check=True)),
                     [sq.tok, ones_f.tok], [B_SS.tok])
            blocks = [[(c0, WB)] for c0 in range(0, D, WB)]
            linear_fm("wom%d" % j if mla else "wod%d" % j, DC, blocks, o_in, [oT], tiles, c_y, wring, p0)
            rstd_from_bank(B_SS, TP, D, rstd)
            for c in range(DC):
                xb = xring.next()
                P.dma("sp", "x%d" % ((xring.i - 1) % len(xring.bufs)), xb.ap[:, 0:TP], xsrc[c * 128:(c + 1) * 128, p0:p0 + TP], reads=[txc[c]], writes=[xb.tok])
                for (o, s, cls) in tiles:
                    lo = o - p0
                    ev("dve", (lambda e, out=yT.ap[:, c, lo:lo + s], g_=vcol(cls, l, 2, c), r=rstd.ap[:, lo:lo + s]:
                               e.scalar_tensor_tensor(out, out, g_, r, ALU.mult, ALU.mult)), [yT, vecs, rstd], [yT])
                ev("pool", (lambda e, out=yT.ap[:, c, 0:TP], x=xb.ap[:, 0:TP]: e.tensor_tensor(out, out, x, ALU.add)), [yT, xb], [yT])
                P.dma("pool", "s_yT", xcur[c * 128:(c + 1) * 128, p0:p0 + TP], yT.ap[:, c, 0:TP], reads=[yT.tok], writes=[txc[c]])
                ss_accum(yT.ap[:, c, 0:TP], [yT], TP, 0, c == 0, c == DC - 1, sqring)
            rstd_from_bank(B_SS, TP, D, rstd)
            for c in range(DC):
                tb = tmpring.next()
                for (o, s, cls) in tiles:
                    lo = o - p0
                    ev("dve", (lambda e, out=tb.ap[:, lo:lo + s], x=yT.ap[:, c, lo:lo + s], a=vcol(cls, l, 3, c), r=rstd.ap[:, lo:lo + s]:
                               e.scalar_tensor_tensor(out, x, a, r, ALU.mult, ALU.mult)), [yT, vecs, rstd], [tb])
                    ev("pool", (lambda e, out=tb.ap[:, lo:lo + s], b=vcol(cls, l, 4, c): e.tensor_scalar(out, out, b, None, ALU.add)), [tb, vecs], [tb])
                if moe:
                    P.op("pe", (lambda e, out=B_RT.ap[0:E, 0:TP], w=wr.ap[:, c, :], r=tb.ap[:, 0:TP], st=(c == 0), sp=(c == DC - 1): e.matmul(out, w, r, start=st, stop=sp)),
                         [wr.tok, tb.tok], [B_RT.tok])
                ev("act", (lambda e, out=oT.ap[:, c, 0:TP], i=tb.ap[:, 0:TP]: e.activation(out=out, in_=i, func=AF.Copy)), [tb], [oT])
            h_in = lambda kc, o, s: oT.ap[:, kc, o:o + s]
            if moe:
                ev("act", (lambda e, out=lgs.ap[:, 0:TP], i=B_RT.ap[0:E, 0:TP]: e.activation(out=out, in_=i, func=AF.Copy)), [B_RT], [lgs])
                for e_ in range(E):
                    pb = mmring.next()
                    P.op("pe", (lambda e, out=pb.ap[:, 0:TP], w=selm.ap[:, e_, :], r=lgs.ap[:, 0:TP]: e.matmul(out, w, r, start=True, stop=True)),
                         [selm.tok, lgs.tok], [pb.tok])
                    ev("act", (lambda e, out=lg.ap[:, e_, 0:TP], i=pb.ap[:, 0:TP]: e.activation(out=out, in_=i, func=AF.Copy)), [pb], [lg])
                ev("dve", lambda e: e.tensor_tensor(m1.ap[:, 0:TP], lg.ap[:, 0, 0:TP], lg.ap[:, 1, 0:TP], ALU.max), [lg], [m1])
                for e_ in range(2, E):
                    ev("dve", (lambda e, i=lg.ap[:, e_, 0:TP]: e.tensor_tensor(m1.ap[:, 0:TP], m1.ap[:, 0:TP], i, ALU.max)), [lg, m1], [m1])
                for e_ in range(E):
                    ev("dve", (lambda e, out=comb.ap[:, e_, 0:TP], i=lg.ap[:, e_, 0:TP]: e.tensor_tensor(out, i, m1.ap[:, 0:TP], ALU.is_equal)), [lg, m1], [comb])
                    ev("dve", (lambda e, out=lg.ap[:, e_, 0:TP], mk=comb.ap[:, e_, 0:TP]: e.scalar_tensor_tensor(out, mk, -1e30, out, ALU.mult, ALU.add)), [lg, comb], [lg])
                ev("dve", lambda e: e.tensor_tensor(m2.ap[:, 0:TP], lg.ap[:, 0, 0:TP], lg.ap[:, 1, 0:TP], ALU.max), [lg], [m2])
                for e_ in range(2, E):
                    ev("dve", (lambda e, i=lg.ap[:, e_, 0:TP]: e.tensor_tensor(m2.ap[:, 0:TP], m2.ap[:, 0:TP], i, ALU.max)), [lg, m2], [m2])
                ev("dve", lambda e: e.tensor_tensor(w1.ap[:, 0:TP], m1.ap[:, 0:TP], m2.ap[:, 0:TP], ALU.subtract), [m1, m2], [w1])
                ev("act", lambda e: e.activation(out=w1.ap[:, 0:TP], in_=w1.ap[:, 0:TP], func=AF.Sigmoid), [w1], [w1])
                ev("dve", lambda e: e.tensor_scalar(m1.ap[:, 0:TP], w1.ap[:, 0:TP], -1.0, 1.0, ALU.mult, ALU.add), [w1], [m1])
                for e_ in range(E):
                    tb = tmpring.next()
                    ev("dve", (lambda e, out=tb.ap[:, 0:TP], i=lg.ap[:, e_, 0:TP]: e.tensor_tensor(out, i, m2.ap[:, 0:TP], ALU.is_equal)), [lg, m2], [tb])
                    ev("dve", (lambda e, out=tb.ap[:, 0:TP]: e.tensor_tensor(out, out, m1.ap[:, 0:TP], ALU.mult)), [tb, m1], [tb])
                    ev("dve", (lambda e, out=comb.ap[:, e_, 0:TP]: e.tensor_tensor(out, out, w1.ap[:, 0:TP], ALU.mult)), [comb, w1], [comb])
                    ev("dve", (lambda e, out=comb.ap[:, e_, 0:TP], t_=tb.ap[:, 0:TP]: e.tensor_tensor(out, out, t_, ALU.add)), [comb, tb], [comb])
            for ex in range(E if moe else 1):
                gname = ("mgu%d_%d" % (j, ex)) if moe else ("wgu%d" % j)
                dname = ("mdn%d_%d" % (j, ex)) if moe else ("wdn%d" % j)

                def c_gu(bi, mi, tile, pb):
                    o, s, cls = tile
                    lo = o - p0
                    if mi == 0:
                        sg = sgr.bufs[bi % 2]
                        ev("act", (lambda e, out=sg.ap[:, lo:lo + s], i=pb.ap[:, 0:s]: e.activation(out=out, in_=i, func=AF.Silu)), [pb], [sg])
                    else:
                        sg = sgr.bufs[bi % 2]
                        if moe:
                            ev("dve", (lambda e, out=sg.ap[:, lo:lo + s], i=pb.ap[:, 0:s]: e.tensor_tensor(out, out, i, ALU.mult)), [pb, sg], [sg])
                            ev("pool", (lambda e, out=aT.ap[:, bi, lo:lo + s], a=sg.ap[:, lo:lo + s], c_=comb.ap[:, ex, lo:lo + s]: e.tensor_tensor(out, a, c_, ALU.mult)), [sg, comb], [aT])
                        else:
                            ev("dve", (lambda e, out=aT.ap[:, bi, lo:lo + s], a=sg.ap[:, lo:lo + s], i=pb.ap[:, 0:s]: e.tensor_tensor(out, a, i, ALU.mult)), [pb, sg], [aT])
                blocks = [[(m * 128, 128), (EDl + m * 128, 128)] for m in range(EC)]
                linear_fm(gname, DC, blocks, h_in, [oT], tiles, c_gu, wring, p0)
                a_in = lambda kc, o, s: aT.ap[:, kc, o:o + s]

                def c_dn(bi, mi, tile, pb):
                    o, s, cls = tile
                    m = bi * (WB // 128) + mi
                    lo = o - p0
                    if ex == 0:
                        ev("act", (lambda e, out=yT.ap[:, m, lo:lo + s], i=pb.ap[:, 0:s]: e.activation(out=out, in_=i, func=AF.Copy)), [pb], [yT])
                    else:
                        ev("dve", (lambda e, out=yT.ap[:, m, lo:lo + s], i=pb.ap[:, 0:s]: e.tensor_tensor(out, out, i, ALU.add)), [pb, yT], [yT])
                blocks = [[(c0, WB)] for c0 in range(0, D, WB)]
                linear_fm(dname, EC, blocks, a_in, [aT], tiles, c_dn, wring, p0)
            for c in range(DC):
                ss_accum(yT.ap[:, c, 0:TP], [yT], TP, 0, c == 0, c == DC - 1, sqring)
            rstd_from_bank(B_SS, TP, D, rstd)
            for c in range(DC):
                xb = xring.next()
                P.dma("sp", "x%d" % ((xring.i - 1) % len(xring.bufs)), xb.ap[:, 0:TP], xcur[c * 128:(c + 1) * 128, p0:p0 + TP], reads=[txc[c]], writes=[xb.tok])
                for (o, s, cls) in tiles:
                    lo = o - p0
                    ev("dve", (lambda e, out=yT.ap[:, c, lo:lo + s], g_=vcol(cls, l, 5, c), r=rstd.ap[:, lo:lo + s]:
                               e.scalar_tensor_tensor(out, out, g_, r, ALU.mult, ALU.mult)), [yT, vecs, rstd], [yT])
                ev("pool", (lambda e, out=yT.ap[:, c, 0:TP], x=xb.ap[:, 0:TP]: e.tensor_tensor(out, out, x, ALU.add)), [yT, xb], [yT])
                P.dma("pool", "s_yT", xdst_final[c * 128:(c + 1) * 128, p0:p0 + TP], yT.ap[:, c, 0:TP], reads=[yT.tok], writes=([txc[c]] if not last_layer else []))
        ar.release()
        P.barrier()

    K.phase_A, K.phase_B, K.phase_C = phase_A, phase_B, phase_C
    xsrc = xT_in
    for l in range(nlayers):
        nkc = phase_A(l, xsrc)
        if stop == "A%d" % l:
            break
        phase_B(l, nkc)
        if stop == "B%d" % l:
            break
        last = (l == nlayers - 1)
        phase_C(l, xsrc, yT_out if last else xcur, last)
        xsrc = xcur
    for name in dbg:
        if name == "QT":
            P.dma("pool", "dbg", dbg[name], QT)
        if name == "KTg":
            P.dma("pool", "dbg", dbg[name], KTg)
        if name == "Vg":
            P.dma("pool", "dbg", dbg[name], Vg)
        if name == "OT":
            P.dma("pool", "dbg", dbg[name], OT)
    fin()
    K.arena_peak = ar.peak
    K.nops = {e: len(P.ops[e]) for e in ENGS}
    return nc


def run(cfg, inp, nlayers=4, debug=(), trace=False):
    maps = prepare_inputs(cfg, inp)
    nc = build(cfg, nlayers, debug)
    res = run_bass_kernel_spmd(nc, maps, core_ids=list(range(8)), trace=trace)
    return res


def assemble(cfg, res):
    B, G, LT, CT, D = cfg["B"], cfg["G"], cfg["LT"], cfg["CT"], cfg["D"]
    out = np.empty((B, G * LT, D), np.float32)
    for c in range(8):
        b, r = c // G, c % G
        yT = np.asarray(res.results[c]["yT"])
        out[b, r * LT:(r + 1) * LT] = yT[:, CT:].T
    return out


_CFG = make_cfg(False)


def kernel(**inputs):
    res = run(_CFG, inputs)
    return assemble(_CFG, res)
```

```python
import math
import types
import numpy as np
import concourse.bass as bass
import concourse.mybir as mybir
from concourse.bass_utils import run_bass_kernel_spmd
from contextlib import ExitStack

F32 = mybir.dt.float32
BF16 = mybir.dt.bfloat16
AF = mybir.ActivationFunctionType
ALU = mybir.AluOpType
AX = mybir.AxisListType

ENGS = ("pe", "act", "dve", "pool", "sp")
EPS = 1e-6


class Tok:
    __slots__ = ("name", "w", "r")

    def __init__(self, name=""):
        self.name = name
        self.w = None
        self.r = []


class Buf:
    __slots__ = ("ap", "tok", "psum")

    def __init__(self, ap, name="", psum=False):
        self.ap = ap
        self.tok = Tok(name)
        self.psum = psum


class Op:
    __slots__ = ("eng", "fn", "waits", "flag", "idx", "dma_inc")

    def __init__(self, eng, fn, idx):
        self.eng = eng
        self.fn = fn
        self.waits = []
        self.flag = False
        self.idx = idx
        self.dma_inc = None


def _snap(fn):
    if fn is None or fn.__closure__ is None:
        return fn
    cells = []
    for c in fn.__closure__:
        try:
            cells.append(types.CellType(c.cell_contents))
        except ValueError:
            cells.append(c)
    return types.FunctionType(fn.__code__, fn.__globals__, fn.__name__, fn.__defaults__, tuple(cells))


class Prog:
    def __init__(self, nc):
        self.nc = nc
        self.ops = {e: [] for e in ENGS}
        self.wm = {e: {} for e in ENGS}
        self.dma_sems = {}
        self.es = ExitStack()

    def _need(self, op, ev):
        if ev is None:
            return
        if ev[0] == "e":
            _, eng, pop = ev
            if eng == "pe" and op.eng == "pe":
                return
            key = ("e", eng)
            cur = self.wm[op.eng].get(key, -1)
            if pop.idx <= cur:
                return
            self.wm[op.eng][key] = pop.idx
            pop.flag = True
            op.waits.append(ev)
        else:
            _, sname, val = ev
            key = ("d", sname)
            cur = self.wm[op.eng].get(key, -1)
            if val <= cur:
                return
            self.wm[op.eng][key] = val
            op.waits.append(ev)

    def _add(self, eng, fn, reads, writes):
        op = Op(eng, fn, len(self.ops[eng]))
        self.ops[eng].append(op)
        for t in reads:
            self._need(op, t.w)
        for t in writes:
            self._need(op, t.w)
            for ev in t.r:
                self._need(op, ev)
        return op

    def op(self, eng, fn, reads=(), writes=()):
        op = self._add(eng, _snap(fn), reads, writes)
        ev = ("e", eng, op)
        for t in reads:
            t.r = [x for x in t.r if not (x[0] == "e" and x[1] == eng)]
            t.r.append(ev)
        for t in writes:
            t.w = ev
            t.r = []
        return op

    def dma(self, q, sem, out, in_, reads=(), writes=(), **kw):
        def fn(e, out=out, in_=in_, kw=kw):
            return e.dma_start(out=out, in_=in_, **kw)
        saved = []
        for t in writes:
            if t.w is not None and t.w[0] == "d" and t.w[1] == sem:
                saved.append((t, t.w))
                t.w = None
        op = self._add(q, fn, reads, writes)
        for t, w in saved:
            t.w = w
        val = self.dma_sems.get(sem, 0) + 16
        self.dma_sems[sem] = val
        op.dma_inc = (sem, 16)
        ev = ("d", sem, val)
        for t in reads:
            t.r = [x for x in t.r if not (x[0] == "d" and x[1] == sem)]
            t.r.append(ev)
        for t in writes:
            t.w = ev
            t.r = []
        return op

    def coll(self, sem, groups, in_, out, reads=(), writes=()):
        def fn(e, in_=in_, out=out, groups=groups):
            return e.collective_compute("AllGather", ALU.bypass, replica_groups=groups,
                                        ins=[in_], outs=[out])
        if not hasattr(self, "cc_tok"):
            self.cc_tok = Tok("cc_serial")
        writes = list(writes) + [self.cc_tok]
        op = self._add("pool", fn, reads, writes)
        val = self.dma_sems.get(sem, 0) + 1
        self.dma_sems[sem] = val
        op.dma_inc = (sem, 1)
        ev = ("d", sem, val)
        for t in reads:
            t.r.append(ev)
        for t in writes:
            t.w = ev
            t.r = []
        return op

    def barrier(self):
        evs = []
        for e in ENGS:
            if e == "sp":
                continue
            for o in reversed(self.ops[e]):
                if o.fn is not None and o.dma_inc is None:
                    evs.append(("e", e, o))
                    break
        for s, v in self.dma_sems.items():
            evs.append(("d", s, v))
        bop = Op("sp", lambda e: e.nop(), len(self.ops["sp"]))
        self.ops["sp"].append(bop)
        for ev in evs:
            self._need(bop, ev)
        bev = ("e", "sp", bop)
        for e in ENGS:
            if e == "sp":
                continue
            o = Op(e, None, len(self.ops[e]))
            self.ops[e].append(o)
            self._need(o, bev)
        for e in ENGS:
            for x in ENGS:
                if self.ops[x]:
                    self.wm[e][("e", x)] = len(self.ops[x]) - 1
            for s_, v in self.dma_sems.items():
                self.wm[e][("d", s_)] = v

    def flush(self, final=False):
        nc = self.nc
        if not hasattr(self, "sem"):
            self.sem = {}
            self.emitted = {e: 0 for e in ENGS}
            self.cum = {e: 0 for e in ENGS}
            self.cnt = {}
        sem = self.sem
        for e in ENGS:
            if ("e", e) not in sem:
                sem[("e", e)] = self.es.enter_context(nc.semaphore("s_" + e))
        for s_ in self.dma_sems:
            if ("d", s_) not in sem:
                sem[("d", s_)] = self.es.enter_context(nc.semaphore("d_" + s_))
        cnt = self.cnt
        for e in ENGS:
            c = self.cum[e]
            for op in self.ops[e][self.emitted[e]:]:
                if op.flag:
                    c += 1
                cnt[(e, op.idx)] = c
            self.cum[e] = c
        ops = self.ops
        dma_sems = self.dma_sems
        start = dict(self.emitted)

        def emit(e, h):
            for op in ops[e][start[e]:]:
                for ev in op.waits:
                    if ev[0] == "e":
                        h.wait_ge(sem[("e", ev[1])], cnt[(ev[1], ev[2].idx)])
                    else:
                        h.wait_ge(sem[("d", ev[1])], ev[2])
                if op.fn is None:
                    continue
                ins = op.fn(h)
                if op.dma_inc is not None:
                    ins.then_inc(sem[("d", op.dma_inc[0])], op.dma_inc[1])
                elif op.flag:
                    ins.then_inc(sem[("e", e)], 1)
            if e == "sp" and final:
                for s_, v in dma_sems.items():
                    h.wait_ge(sem[("d", s_)], v)

        with nc.Block() as block:
            @block.tensor
            def _(h):
                emit("pe", h)

            @block.scalar
            def _(h):
                emit("act", h)

            @block.vector
            def _(h):
                emit("dve", h)

            @block.gpsimd
            def _(h):
                emit("pool", h)

            @block.sync
            def _(h):
                emit("sp", h)
        for e in ENGS:
            self.emitted[e] = len(self.ops[e])

    def finalize(self):
        self.flush(final=True)
        self.es.close()


class Arena:
    def __init__(self, nc, P):
        self.nc = nc
        self.P = P
        self.stacks = [P.es]
        self.n = 0
        self.used = [0]
        self.peak = 0

    def alloc(self, shape_free, dtype, parts=128, name="t"):
        shape_free = [int(s) for s in shape_free]
        self.n += 1
        t = self.stacks[-1].enter_context(self.nc.sbuf_tensor("%s_%d" % (name, self.n), [parts] + shape_free, dtype))
        self.used[-1] += int(np.prod(shape_free)) * (4 if dtype == F32 else 2)
        self.peak = max(self.peak, sum(self.used))
        return t[:]

    def buf(self, shape_free, dtype, name="", parts=128):
        return Buf(self.alloc(shape_free, dtype, parts, name or "t"), name)

    def mark(self):
        self.stacks.append(ExitStack())
        self.used.append(0)

    def release(self):
        self.P.barrier()
        self.P.flush()
        self.stacks.pop().close()
        self.used.pop()


class Ring:
    def __init__(self, bufs):
        self.bufs = bufs
        self.i = 0

    def next(self):
        b = self.bufs[self.i % len(self.bufs)]
        self.i += 1
        return b


def make_cfg(small=False):
    if small:
        c = dict(D=512, LT=512, CT=64, L0=256, LP=256, H=4, QR=256, KVR=128, H2=2, F=384, E=8, ED=128)
    else:
        c = dict(D=4096, LT=2048, CT=64, L0=256, LP=448, H=32, QR=1024, KVR=512, H2=16, F=3072, E=8, ED=1024)
    c["L"] = 4
    c["G"] = 4
    c["NC"] = 8
    c["B"] = 2
    c["T"] = c["CT"] + c["LT"]
    c["DC"] = c["D"] // 128
    c["GRID_W"] = 64
    c["S"] = c["LT"] * c["G"]
    c["C"] = c["CT"] * c["G"]
    assert c["H"] * 128 == c["D"] and c["H2"] * 256 == c["D"]
    assert (c["LT"] - c["L0"]) % c["LP"] == 0
    assert (6 * c["DC"]) % 8 == 0
    assert c["CT"] == 64 and c["T"] % 64 == 0
    c["CPR"] = 6 * c["DC"] // 8
    return c


def weight_list(cfg):
    D, QR, KVR, H, F, E, ED = cfg["D"], cfg["QR"], cfg["KVR"], cfg["H"], cfg["F"], cfg["E"], cfg["ED"]
    ws = []
    for j in range(2):
        ws += [("wdq%d" % j, D, QR), ("wuq%d" % j, QR, H * 192), ("wdkv%d" % j, D, KVR + 128),
               ("wukv%d" % j, KVR, H * 256), ("wom%d" % j, D, D),
               ("wgu%d" % j, D, 2 * F), ("wdn%d" % j, F, D)]
    for j in range(2):
        ws += [("wqkv%d" % j, D, 3 * D), ("wod%d" % j, D, D)]
        for e in range(E):
            ws += [("mgu%d_%d" % (j, e), D, 2 * ED), ("mdn%d_%d" % (j, e), ED, D)]
    return ws


import os as _os
CC_LIMIT = int(_os.environ.get("CC_LIMIT", 512 * 1024))


def colblock(Kd, N):
    lim = CC_LIMIT // ((Kd // 8) * 2)
    if N <= lim:
        return N
    best = 128
    for cw in range(128, lim + 1, 128):
        if N % cw == 0:
            best = cw
    return best


def passes_of(cfg):
    CT, L0, LP, LT = cfg["CT"], cfg["L0"], cfg["LP"], cfg["LT"]
    ps = [[(0, CT, 1), (CT, L0, 0)]]
    o = CT + L0
    while o < CT + LT:
        ps.append([(o, LP, 0)])
        o += LP
    return ps


def subtiles(tiles):
    out = []
    for (o, s, c) in tiles:
        a = 0
        while a < s:
            n = min(128, s - a)
            out.append((o + a, n))
            a += n
    return out


def rope_tables_np(cfg, r, dim):
    T, CT, LT, GW = cfg["T"], cfg["CT"], cfg["LT"], cfg["GRID_W"]
    half = dim // 2
    nfreq = half // 2
    inv = (10000.0 ** (-np.arange(0, half, 2, dtype=np.float32) / np.float32(half))).astype(np.float32)
    t = np.arange(r * LT, (r + 1) * LT)
    row = (t // GW).astype(np.float32)
    col = (t % GW).astype(np.float32)
    cos = np.ones((128, T), np.float32)
    sin = np.zeros((128, T), np.float32)
    for p in range(128):
        f = p % dim
        s = f // half
        g = f % half
        i = g % nfreq
        pos = row if s == 0 else col
        ang = (pos * inv[i]).astype(np.float32)
        cos[p, CT:] = np.cos(ang)
        sin[p, CT:] = np.sin(ang)
    return cos, sin


def perm_np(dim):
    half = dim // 2
    nfreq = half // 2
    m = np.zeros((128, 128), np.float32)
    for p in range(128):
        base = p - (p % dim)
        f = p % dim
        s = f // half
        g = f % half
        part = g // nfreq
        if part == 0:
            k = p + nfreq
            m[k, p] = -1.0
        else:
            k = p - nfreq
            m[k, p] = 1.0
    return m


def cols_np(v):
    return np.ascontiguousarray(v.reshape(-1, 128).T)


def prepare_inputs(cfg, inp):
    D, T, CT, LT, DC, L, NCR, G = cfg["D"], cfg["T"], cfg["CT"], cfg["LT"], cfg["DC"], cfg["L"], cfg["NC"], cfg["G"]
    H, QR, KVR, H2, F, E, ED, CPR = cfg["H"], cfg["QR"], cfg["KVR"], cfg["H2"], cfg["F"], cfg["E"], cfg["ED"], cfg["CPR"]
    f32 = np.float32
    x = np.asarray(inp["x"], f32)
    ctx = np.asarray(inp["ctx"], f32)
    cvec = np.stack([np.asarray(inp["c"], f32)[0], np.asarray(inp["c"], f32)[1], np.asarray(inp["c_ctx"], f32)], 0)
    cv = np.ascontiguousarray(cvec.reshape(3, DC, 128).transpose(2, 1, 0))
    ada_w = np.asarray(inp["ada_w"], f32)
    ada_b = np.asarray(inp["ada_b"], f32)
    ng = np.ascontiguousarray(np.asarray(inp["norm_g"], f32).reshape(L, 4, DC, 128).transpose(3, 0, 1, 2))
    full = {}
    for j in range(2):
        wuq = np.asarray(inp["mla_w_uq"], f32)[j].reshape(QR, H, 192)
        full["wdq%d" % j] = np.asarray(inp["mla_w_dq"], f32)[j]
        full["wuq%d" % j] = np.concatenate([wuq[:, :, :128].reshape(QR, H * 128), wuq[:, :, 128:].reshape(QR, H * 64)], 1)
        wdkv = np.asarray(inp["mla_w_dkv"], f32)[j]
        full["wdkv%d" % j] = np.concatenate([wdkv[:, :KVR], wdkv[:, KVR:], wdkv[:, KVR:]], 1)
        wukv = np.asarray(inp["mla_w_ukv"], f32)[j].reshape(KVR, H, 256)
        full["wukv%d" % j] = np.concatenate([wukv[:, :, :128].reshape(KVR, H * 128), wukv[:, :, 128:].reshape(KVR, H * 128)], 1)
        full["wom%d" % j] = np.asarray(inp["mla_w_o"], f32)[j]
        full["wgu%d" % j] = np.asarray(inp["ffn_w_gu"], f32)[j]
        full["wdn%d" % j] = np.asarray(inp["ffn_w_down"], f32)[j]
        full["wqkv%d" % j] = np.asarray(inp["diff_w_qkv"], f32)[j]
        full["wod%d" % j] = np.asarray(inp["diff_w_o"], f32)[j]
        for e in range(E):
            full["mgu%d_%d" % (j, e)] = np.asarray(inp["moe_w_gu"], f32)[j, e]
            full["mdn%d_%d" % (j, e)] = np.asarray(inp["moe_w_down"], f32)[j, e]
    pm = perm_np(64)
    pd = perm_np(128)
    maps = []
    for c in range(NCR):
        b, r = c // G, c % G
        sh = (c % 4) * 2 + c // 4
        m = {}
        xt = np.concatenate([ctx[b, r * CT:(r + 1) * CT], x[b, r * LT:(r + 1) * LT]], 0)
        m["xT"] = np.ascontiguousarray(xt.T)
        m["cv"] = cv
        m["adaw"] = np.ascontiguousarray(ada_w[:, :, sh * CPR * 128:(sh + 1) * CPR * 128])
        m["adab"] = np.ascontiguousarray(ada_b[:, sh * CPR * 128:(sh + 1) * CPR * 128].reshape(L, CPR, 128).transpose(2, 0, 1))
        sel = np.zeros((128, 3), f32)
        sel[:, b] = 1.0
        m["sel"] = sel
        m["ng"] = ng
        cm, sm = rope_tables_np(cfg, r, 64)
        cd, sd = rope_tables_np(cfg, r, 128)
        m["ropeM"] = np.ascontiguousarray(np.stack([cm, sm], 1))
        m["ropeD"] = np.ascontiguousarray(np.stack([cd, sd], 1))
        m["perms"] = np.ascontiguousarray(np.stack([pm, pd], 1))
        sm_ = np.zeros((E, E, 128), f32)
        for e_ in range(E):
            sm_[e_, e_, :] = 1.0
        m["selm"] = sm_.reshape(E, E * 128)
        for j in range(2):
            m["qn%d" % j] = cols_np(np.asarray(inp["mla_q_norm"], f32)[j])
            m["kvn%d" % j] = cols_np(np.asarray(inp["mla_kv_norm"], f32)[j])
            m["lam%d" % j] = np.ascontiguousarray(np.broadcast_to(np.asarray(inp["diff_lambda"], f32)[j][None], (128, 4, 128)))
            m["sln%d" % j] = cols_np(np.asarray(inp["diff_subln"], f32)[j])
            m["wr%d" % j] = np.ascontiguousarray(np.asarray(inp["moe_router"], f32)[j].reshape(DC, 128, E).transpose(1, 0, 2))
        for name, K, N in weight_list(cfg):
            m[name] = np.ascontiguousarray(full[name][sh * (K // 8):(sh + 1) * (K // 8)])
        maps.append(m)
    return maps


class K:
    pass


def build(cfg, nlayers=4, debug=(), stop=None):
    nc = bass.Bass("TRN2", target_bir_lowering=False)
    P = Prog(nc)
    es = P.es
    D, T, CT, LT, DC, L, G = cfg["D"], cfg["T"], cfg["CT"], cfg["LT"], cfg["DC"], cfg["L"], cfg["G"]
    H, QR, KVR, H2, F, E, ED, CPR = cfg["H"], cfg["QR"], cfg["KVR"], cfg["H2"], cfg["F"], cfg["E"], cfg["ED"], cfg["CPR"]
    QC, KVC = QR // 128, KVR // 128
    PASSES = passes_of(cfg)
    TPMAX = max(sum(s for (_, s, _) in p) for p in PASSES)
    WB = 256
    KCMAX = max(DC, F // 128, QC, KVC, ED // 128)

    def ext_in(name, shape, dt=F32):
        return nc.dram_tensor(name, list(shape), dt, kind="ExternalInput").ap()

    def dram(name, shape, dt):
        return nc.dram_tensor(name, list(shape), dt).ap()

    xT_in = ext_in("xT", [D, T])
    cv_in = ext_in("cv", [128, DC, 3])
    adaw_in = ext_in("adaw", [L, D, CPR * 128])
    adab_in = ext_in("adab", [128, L, CPR])
    sel_in = ext_in("sel", [128, 3])
    ng_in = ext_in("ng", [128, L, 4, DC])
    ropeM_in = ext_in("ropeM", [128, 2, T])
    ropeD_in = ext_in("ropeD", [128, 2, T])
    perms_in = ext_in("perms", [128, 2, 128])
    selm_in = ext_in("selm", [E, E * 128])
    small_in = {}
    for j in range(2):
        small_in["qn%d" % j] = ext_in("qn%d" % j, [128, QC])
        small_in["kvn%d" % j] = ext_in("kvn%d" % j, [128, KVC])
        small_in["lam%d" % j] = ext_in("lam%d" % j, [128, 4, 128])
        small_in["sln%d" % j] = ext_in("sln%d" % j, [128, 2])
        small_in["wr%d" % j] = ext_in("wr%d" % j, [128, DC, E])
    wl = weight_list(cfg)
    w_in, w_sh, w_full, w_tok, w_tmp, w_cw = {}, {}, {}, {}, {}, {}
    for name, Kd, N in wl:
        cw = colblock(Kd, N)
        ncb = N // cw
        w_cw[name] = (cw, ncb, Kd)
        w_in[name] = ext_in(name, [Kd // 8, N])
        w_sh[name] = dram(name + "_sh", [ncb * (Kd // 8), cw], BF16)
        w_full[name] = dram(name + "_bf", [ncb * Kd, cw], BF16)
        w_tmp[name] = dram(name + "_tmp", [ncb * (Kd // 4), cw], BF16)
        w_tok[name] = Tok(name)
    yT_out = nc.dram_tensor("yT", [D, T], F32, kind="ExternalOutput").ap()

    xcur = dram("xcur", [D, T], F32)
    modsh = dram("modsh", [128, L * CPR * 3], F32)
    modall = dram("modall", [8 * 128, L * CPR * 3], F32)
    modtmp = dram("modtmp", [2 * 128, L * CPR * 3], F32)
    NQC = max(H + H // 2, 2 * H2)
    NKC = max(H + 1, 2 * H2)
    QT = dram("QT", [NQC * 128, T], BF16)
    KTl = dram("KTl", [NKC * 128, T], BF16)
    KTg = dram("KTg", [G * NKC * 128, T], BF16)
    Vl = dram("Vl", [T, D], BF16)
    Vg = dram("Vg", [G * T, D], BF16)
    OT = dram("OT", [D, T], BF16)
    txc = [Tok("xc%d" % c) for c in range(DC)]
    dbg = {}
    for name, shape, dt in debug:
        dbg[name] = nc.dram_tensor("dbg_" + name, list(shape), dt, kind="ExternalOutput").ap()

    ar = Arena(nc, P)
    banks = [Buf(es.enter_context(nc.psum_tensor("pb%d" % i, [128, 512], F32))[:], "pb%d" % i, True) for i in range(8)]

    ones_f = ar.buf((128,), F32, "ones_f")
    ones_b = ar.buf((128,), BF16, "ones_b")
    perm_b = ar.buf((2, 128), BF16, "perm_b")
    modreg = ar.buf((2, L, 6 * DC), F32, "modreg")
    ngc = ar.buf((L, 4, DC), F32, "ngc")
    vecs = ar.buf((2, L, 6, DC), F32, "vecs")
    P.op("pool", lambda e: e.memset(ones_f.ap, 1.0), writes=[ones_f.tok])
    P.op("pool", lambda e: e.memset(ones_b.ap, 1.0), writes=[ones_b.tok])
    P.dma("pool", "c_perm", perm_b.ap, perms_in, writes=[perm_b.tok])
    P.dma("sp", "c_ng", ngc.ap, ng_in, writes=[ngc.tok])

    def ev(eng, fn, reads, writes):
        rd = [b.tok for b in reads if not b.psum]
        wr = [b.tok for b in writes] + [b.tok for b in reads if b.psum]
        return P.op(eng, fn, rd, wr)

    PAIRS = [[0, 4], [1, 5], [2, 6], [3, 7]]
    QUADS = [[0, 1, 2, 3], [4, 5, 6, 7]]

    def allgather8(src, tmp, dst, reads, tout):
        tt = Tok("ag_tmp")
        P.coll("cc", PAIRS, src.opt(), tmp.opt(), reads=reads, writes=[tt])
        P.coll("cc", QUADS, tmp.opt(), dst.opt(), reads=[tt], writes=[tout])

    def fin():
        P.flush(final=True)
        while len(ar.stacks) > 1:
            ar.stacks.pop().close()
        P.es.close()

    ar.mark()
    cvt = ar.buf((DC, 3), F32, "cvt")
    svt = ar.buf((DC, 3), F32, "svt")
    adab_t = ar.buf((L, CPR), F32, "adab")
    modloc = ar.buf((L, CPR, 3), F32, "modloc")
    selt = ar.buf((3,), F32, "sel")
    P.dma("sp", "c_cv", cvt.ap, cv_in, writes=[cvt.tok])
    P.dma("sp", "c_adab", adab_t.ap, adab_in, writes=[adab_t.tok])
    P.dma("sp", "c_sel", selt.ap, sel_in, writes=[selt.tok])
    ev("act", lambda e: e.activation(out=svt.ap, in_=cvt.ap, func=AF.Silu), [cvt], [svt])
    AWB = 512 if CPR * 128 >= 512 else CPR * 128
    nab = CPR * 128 // AWB
    KCH = 8 if DC >= 8 else DC
    awr = Ring([ar.buf((DC, AWB), F32, "aw%d" % i) for i in range(2)])
    adaw_v = adaw_in.rearrange("l (kc p) n -> l p kc n", p=128)
    for l in range(L):
        for ab in range(nab):
            wb = awr.next()
            for k0 in range(0, DC, KCH):
                P.dma("sp", "aw%d" % ((awr.i - 1) % 2), wb.ap[:, k0:k0 + KCH, :],
                      adaw_v[l, :, k0:k0 + KCH, ab * AWB:(ab + 1) * AWB], writes=[wb.tok])
            for mi in range(AWB // 128):
                ck = ab * (AWB // 128) + mi
                pb = banks[(l * nab * 4 + ab * 4 + mi) % 4]
                for kc in range(DC):
                    P.op("pe", (lambda e, o=pb.ap[:, 0:3], w=wb.ap[:, kc, mi * 128:(mi + 1) * 128], r=svt.ap[:, kc, :], s=(kc == 0), t=(kc == DC - 1):
                                e.matmul(o, w, r, start=s, stop=t)),
                         [wb.tok, svt.tok], [pb.tok])
                ev("dve", (lambda e, o=modloc.ap[:, l, ck, :], i=pb.ap[:, 0:3], b=adab_t.ap[:, l, ck:ck + 1]:
                           e.tensor_scalar(o, i, b, None, ALU.add)), [pb, adab_t], [modloc])
    if stop == "0a1":
        fin()
        return nc
    tmod = Tok("modsh")
    P.dma("pool", "s_mod", modsh, modloc.ap.rearrange("p l c j -> p (l c j)"), reads=[modloc.tok], writes=[tmod])
    tmodall = Tok("modall")
    allgather8(modsh, modtmp, modall, [tmod], tmodall)
    if stop == "0a2":
        fin()
        return nc
    modc = ar.buf((8, L, CPR, 3), F32, "modc")
    P.dma("pool", "c_modc", modc.ap.rearrange("p r l c j -> p r (l c j)"),
          modall.rearrange("(r p) f -> p r f", p=128), reads=[tmodall], writes=[modc.tok])
    if stop == "0a3":
        fin()
        return nc
    mr_lat = modreg.ap[:, 0].rearrange("p l (r c) -> p l r c", r=8)
    mr_ctx = modreg.ap[:, 1].rearrange("p l (r c) -> p l r c", r=8)
    for l in range(L):
        src = lambda j: modc.ap[:, :, l, :, j]
        ev("dve", (lambda e, o=mr_ctx[:, l], i=src(2): e.tensor_copy(o, i)), [modc], [modreg])
        ev("dve", (lambda e, o=mr_lat[:, l], i=src(0), s=selt.ap[:, 0:1]: e.tensor_scalar(o, i, s, None, ALU.mult)), [modc, selt], [modreg])
        ev("dve", (lambda e, o=mr_lat[:, l], i=src(1), s=selt.ap[:, 1:2]: e.scalar_tensor_tensor(o, i, s, o, ALU.mult, ALU.add)), [modc, selt, modreg], [modreg])
    if stop == "0a4":
        fin()
        return nc
    for cls in range(2):
        for l in range(L):
            md = lambda m: modreg.ap[:, cls, l, m * DC:(m + 1) * DC]
            g = lambda i: ngc.ap[:, l, i, :]
            v = lambda i: vecs.ap[:, cls, l, i, :]
            ev("dve", (lambda e, o=v(0), i=md(1), g_=g(0): e.scalar_tensor_tensor(o, i, 1.0, g_, ALU.add, ALU.mult)), [modreg, ngc], [vecs])
            ev("dve", (lambda e, o=v(1), i=md(0): e.tensor_copy(o, i)), [modreg], [vecs])
            ev("dve", (lambda e, o=v(2), i=md(2), g_=g(1): e.tensor_tensor(o, i, g_, ALU.mult)), [modreg, ngc], [vecs])
            ev("dve", (lambda e, o=v(3), i=md(4), g_=g(2): e.scalar_tensor_tensor(o, i, 1.0, g_, ALU.add, ALU.mult)), [modreg, ngc], [vecs])
            ev("dve", (lambda e, o=v(4), i=md(3): e.tensor_copy(o, i)), [modreg], [vecs])
            ev("dve", (lambda e, o=v(5), i=md(5), g_=g(3): e.tensor_tensor(o, i, g_, ALU.mult)), [modreg, ngc], [vecs])
    if "vecs" in dbg:
        P.dma("pool", "dbg", dbg["vecs"], vecs.ap.rearrange("p a l i c -> p (a l i c)"), reads=[vecs.tok])
    ar.release()
    if stop == "0a":
        fin()
        return nc

    used = set()
    for li in range(nlayers):
        j = li // 2
        if li % 2 == 0:
            used |= {"wdq%d" % j, "wuq%d" % j, "wdkv%d" % j, "wukv%d" % j, "wom%d" % j, "wgu%d" % j, "wdn%d" % j}
        else:
            used |= {"wqkv%d" % j, "wod%d" % j} | {"mgu%d_%d" % (j, e) for e in range(E)} | {"mdn%d_%d" % (j, e) for e in range(E)}
    for name, Kd, N in wl:
        if name not in used:
            continue
        rows = Kd // 8
        cw, ncb, _ = w_cw[name]
        for cb in range(ncb):
            tsh = Tok(name + "_sh")
            for r0 in range(0, rows, 128):
                r1 = min(rows, r0 + 128)
                P.dma("pool", "wcast", w_sh[name][cb * rows + r0:cb * rows + r1, :], w_in[name][r0:r1, cb * cw:(cb + 1) * cw], writes=[tsh])
            allgather8(w_sh[name][cb * rows:(cb + 1) * rows, :], w_tmp[name][cb * 2 * rows:(cb + 1) * 2 * rows, :],
                       w_full[name][cb * Kd:(cb + 1) * Kd, :], [tsh], w_tok[name])
    P.barrier()
    if stop == "0b":
        fin()
        return nc

    mmring = Ring(banks[0:4])
    B_SS, B_AUX, B_TM, B_RT = banks[4], banks[5], banks[6], banks[7]

    def vcol(cls, l, i, c):
        return vecs.ap[:, cls, l, i, c:c + 1]

    def load_weight_block(Wname, KC, ranges, wb, slot_sem):
        cw, ncb, Kd = w_cw[Wname]
        off = 0
        for (c0, n) in ranges:
            a = c0
            while a < c0 + n:
                cb = a // cw
                nn = min(c0 + n, (cb + 1) * cw) - a
                Wv = w_full[Wname][cb * Kd:(cb + 1) * Kd, :].rearrange("(kc p) n -> p kc n", p=128)
                for k0 in range(0, KC, 8):
                    k1 = min(KC, k0 + 8)
                    P.dma("sp", slot_sem, wb.ap[:, k0:k1, off:off + nn], Wv[:, k0:k1, a - cb * cw:a - cb * cw + nn],
                          reads=[w_tok[Wname]], writes=[wb.tok])
                off += nn
                a += nn

    def linear_fm(Wname, KC, blocks, in_ap, in_bufs, tiles, consumer, wring, local0=0):
        nb = len(blocks)
        wbs = [None] * nb

        def load(bi):
            wb = wring.next()
            wbs[bi] = wb
            load_weight_block(Wname, KC, blocks[bi], wb, "w%d" % ((wring.i - 1) % len(wring.bufs)))
        load(0)
        for bi in range(nb):
            if bi + 1 < nb:
                load(bi + 1)
            wb = wbs[bi]
            nm = sum(n for (_, n) in blocks[bi]) // 128
            for mi in range(nm):
                for (o, s, cls) in tiles:
                    pb = mmring.next()
                    for kc in range(KC):
                        P.op("pe", (lambda e, out=pb.ap[:, 0:s], w=wb.ap[:, kc, mi * 128:(mi + 1) * 128], r=in_ap(kc, o - local0, s), st=(kc == 0), sp=(kc == KC - 1):
                                    e.matmul(out, w, r, start=st, stop=sp)),
                             [wb.tok] + [b.tok for b in in_bufs], [pb.tok])
                    consumer(bi, mi, (o, s, cls), pb)

    def linear_tm(Wname, KC, blocks, in_ap, in_bufs, stiles, consumer, wring, local0=0):
        nb = len(blocks)
        wbs = [None] * nb

        def load(bi):
            wb = wring.next()
            wbs[bi] = wb
            load_weight_block(Wname, KC, blocks[bi], wb, "w%d" % ((wring.i - 1) % len(wring.bufs)))
        load(0)
        for bi in range(nb):
            if bi + 1 < nb:
                load(bi + 1)
            wb = wbs[bi]
            ncol = sum(n for (_, n) in blocks[bi])
            for (o, s) in stiles:
                pb = mmring.next()
                for kc in range(KC):
                    P.op("pe", (lambda e, out=pb.ap[0:s, 0:ncol], a=in_ap(kc, o - local0, s), w=wb.ap[:, kc, 0:ncol], st=(kc == 0), sp=(kc == KC - 1):
                                e.matmul(out, a, w, start=st, stop=sp)),
                         [wb.tok] + [b.tok for b in in_bufs], [pb.tok])
                consumer(bi, (o, s), pb, ncol)

    def rstd_from_bank(bank, n, dim, out):
        ev("dve", (lambda e, o=out.ap[:, 0:n], i=bank.ap[:, 0:n]: e.tensor_scalar(o, i, 1.0 / dim, EPS, ALU.mult, ALU.add)), [bank], [out])
        ev("act", (lambda e, o=out.ap[:, 0:n]: e.activation(out=o, in_=o, func=AF.Sqrt)), [out], [out])
        ev("dve", (lambda e, o=out.ap[:, 0:n]: e.reciprocal(o, o)), [out], [out])

    def ss_accum(src_ap, src_bufs, n, col0, first, last, sqring):
        sq = sqring.next()
        sq_ap = sq.ap
        ev("act", (lambda e, o=sq_ap[:, 0:n], i=src_ap: e.activation(out=o, in_=i, func=AF.Square)), src_bufs, [sq])
        P.op("pe", (lambda e, o=B_SS.ap[:, col0:col0 + n], r=sq_ap[:, 0:n], st=(first and col0 == 0), sp=last: e.matmul(o, ones_f.ap, r, start=st, stop=sp, skip_group_check=True)),
             [sq.tok, ones_f.tok], [B_SS.tok])

    def prenorm(xsrc, tiles, TP, p0, l, ai, bi_, hT, xring, sqring, rstd, tmpring, extra=None):
        for c in range(DC):
            xb = xring.next()
            P.dma("sp", "x%d" % ((xring.i - 1) % len(xring.bufs)), xb.ap[:, 0:TP], xsrc[c * 128:(c + 1) * 128, p0:p0 + TP], reads=[txc[c]], writes=[xb.tok])
            ss_accum(xb.ap[:, 0:TP], [xb], TP, 0, c == 0, c == DC - 1, sqring)
        rstd_from_bank(B_SS, TP, D, rstd)
        for c in range(DC):
            xb = xring.next()
            P.dma("sp", "x%d" % ((xring.i - 1) % len(xring.bufs)), xb.ap[:, 0:TP], xsrc[c * 128:(c + 1) * 128, p0:p0 + TP], reads=[txc[c]], writes=[xb.tok])
            tb = tmpring.next()
            for (o, s, cls) in tiles:
                lo = o - p0
                ev("dve", (lambda e, out=tb.ap[:, lo:lo + s], x=xb.ap[:, lo:lo + s], a=vcol(cls, l, ai, c), r=rstd.ap[:, lo:lo + s]:
                           e.scalar_tensor_tensor(out, x, a, r, ALU.mult, ALU.mult)), [xb, vecs, rstd], [tb])
                ev("pool", (lambda e, out=tb.ap[:, lo:lo + s], b=vcol(cls, l, bi_, c): e.tensor_scalar(out, out, b, None, ALU.add)), [tb, vecs], [tb])
            if extra is not None:
                extra(c, tb)
            ev("act", (lambda e, out=hT.ap[:, c, 0:TP], i=tb.ap[:, 0:TP]: e.activation(out=out, in_=i, func=AF.Copy)), [tb], [hT])

    stg_cnt = [0]

    def store(dst, src_buf, src_ap, writes=()):
        P.dma("pool", "s_" + src_buf.tok.name, dst, src_ap, reads=[src_buf.tok], writes=list(writes))

    def rope_store(pb, o, s, p0, rtab, pidx, premul, stg_ring, tmpring, dst):
        lo = o - p0
        qraw = stg_ring.next()
        if premul is not None:
            ev("dve", (lambda e, out=qraw.ap[:, 0:s], i=pb.ap[:, 0:s], r=premul: e.tensor_tensor(out, i, r, ALU.mult)), [pb], [qraw])
        else:
            ev("act", (lambda e, out=qraw.ap[:, 0:s], i=pb.ap[:, 0:s]: e.activation(out=out, in_=i, func=AF.Copy)), [pb], [qraw])
        P.op("pe", (lambda e, out=B_AUX.ap[:, 0:s], r=qraw.ap[:, 0:s]: e.matmul(out, perm_b.ap[:, pidx, :], r, start=True, stop=True)),
             [qraw.tok, perm_b.tok], [B_AUX.tok])
        t1 = tmpring.next()
        t2 = tmpring.next()
        ev("dve", (lambda e, out=t1.ap[:, 0:s], i=qraw.ap[:, 0:s], c_=rtab.ap[:, 0, lo:lo + s]: e.tensor_tensor(out, i, c_, ALU.mult)), [qraw, rtab], [t1])
        ev("dve", (lambda e, out=t2.ap[:, 0:s], i=B_AUX.ap[:, 0:s], s_=rtab.ap[:, 1, lo:lo + s]: e.tensor_tensor(out, i, s_, ALU.mult)), [B_AUX, rtab], [t2])
        qo = stg_ring.next()
        ev("pool", (lambda e, out=qo.ap[:, 0:s], a=t1.ap[:, 0:s], b=t2.ap[:, 0:s]: e.tensor_tensor(out, a, b, ALU.add)), [t1, t2], [qo])
        store(dst, qo, qo.ap[:, 0:s])

    def phase_A(l, xsrc):
        mla = (l % 2 == 0)
        j = l // 2
        ar.mark()
        hT = ar.buf((DC, TPMAX), BF16, "hT")
        wring = Ring([ar.buf((KCMAX, WB), BF16, "wb%d" % i) for i in range(2)])
        xring = Ring([ar.buf((TPMAX,), F32, "xb%d" % i) for i in range(3)])
        sqring = Ring([ar.buf((TPMAX,), F32, "sq%d" % i) for i in range(2)])
        tmpring = Ring([ar.buf((TPMAX,), F32, "tmp%d" % i) for i in range(4)])
        stg = Ring([ar.buf((512,), BF16, "stg%d" % i) for i in range(6)])
        rstd = ar.buf((TPMAX,), F32, "rstd")
        rtab = ar.buf((2, TPMAX), F32, "rtab")
        if mla:
            qaT = ar.buf((QC, TPMAX), BF16, "qaT")
            ckT = ar.buf((KVC, TPMAX), BF16, "ckT")
            cksq = ar.buf((KVC, TPMAX), F32, "cksq")
            rq = ar.buf((TPMAX,), F32, "rq")
            rkv = ar.buf((TPMAX,), F32, "rkv")
            rkvc = ar.buf((8,), F32, "rkvc")
            qn = ar.buf((QC,), F32, "qn")
            kvn = ar.buf((KVC,), F32, "kvn")
            P.dma("sp", "c_qn", qn.ap, small_in["qn%d" % j], writes=[qn.tok])
            P.dma("sp", "c_kvn", kvn.ap, small_in["kvn%d" % j], writes=[kvn.tok])
        for tiles in PASSES:
            p0 = tiles[0][0]
            TP = sum(s for (_, s, _) in tiles)
            P.dma("sp", "c_rt", rtab.ap[:, :, 0:TP], (ropeM_in if mla else ropeD_in)[:, :, p0:p0 + TP], writes=[rtab.tok])
            prenorm(xsrc, tiles, TP, p0, l, 0, 1, hT, xring, sqring, rstd, tmpring)
            h_in = lambda kc, o, s: hT.ap[:, kc, o:o + s]
            if mla:
                def c_qa(bi, mi, tile, pb):
                    o, s, cls = tile
                    m = bi * (WB // 128) + mi
                    lo = o - p0
                    ss_accum(pb.ap[:, 0:s], [pb], s, lo, m == 0, m == QC - 1, sqring)
                    ev("dve", (lambda e, out=qaT.ap[:, m, lo:lo + s], i=pb.ap[:, 0:s], g=qn.ap[:, m:m + 1]: e.tensor_scalar(out, i, g, None, ALU.mult)), [pb, qn], [qaT])
                blocks = [[(c0, min(WB, QR - c0))] for c0 in range(0, QR, WB)]
                linear_fm("wdq%d" % j, DC, blocks, h_in, [hT], tiles, c_qa, wring, p0)
                rstd_from_bank(B_SS, TP, QR, rq)
                qa_in = lambda kc, o, s: qaT.ap[:, kc, o:o + s]

                def c_q(bi, mi, tile, pb):
                    o, s, cls = tile
                    m = bi * (WB // 128) + mi
                    lo = o - p0
                    if m < H:
                        sb = stg.next()
                        ev("dve", (lambda e, out=sb.ap[:, 0:s], i=pb.ap[:, 0:s], r=rq.ap[:, lo:lo + s]: e.tensor_tensor(out, i, r, ALU.mult)), [pb, rq], [sb])
                        store(QT[m * 128:(m + 1) * 128, o:o + s], sb, sb.ap[:, 0:s])
                    else:
                        rope_store(pb, o, s, p0, rtab, 0, rq.ap[:, lo:lo + s], stg, tmpring, QT[m * 128:(m + 1) * 128, o:o + s])
                NQ = H * 192
                blocks = [[(c0, min(WB, NQ - c0))] for c0 in range(0, NQ, WB)]
                linear_fm("wuq%d" % j, QC, blocks, qa_in, [qaT, rq], tiles, c_q, wring, p0)
                def c_kv(bi, mi, tile, pb):
                    o, s, cls = tile
                    m = bi * (WB // 128) + mi
                    lo = o - p0
                    if m < KVC:
                        ev("act", (lambda e, out=cksq.ap[:, m, lo:lo + s], i=pb.ap[:, 0:s]: e.activation(out=out, in_=i, func=AF.Square)), [pb], [cksq])
                        P.op("pe", (lambda e, out=B_SS.ap[:, lo:lo + s], r=cksq.ap[:, m, lo:lo + s], st=(m == 0 and lo == 0), sp=(m == KVC - 1): e.matmul(out, ones_f.ap, r, start=st, stop=sp, skip_group_check=True)),
                             [cksq.tok, ones_f.tok], [B_SS.tok])
                        ev("dve", (lambda e, out=ckT.ap[:, m, lo:lo + s], i=pb.ap[:, 0:s], g=kvn.ap[:, m:m + 1]: e.tensor_scalar(out, i, g, None, ALU.mult)), [pb, kvn], [ckT])
                    else:
                        rope_store(pb, o, s, p0, rtab, 0, None, stg, tmpring, KTl[H * 128:(H + 1) * 128, o:o + s])
                NKV = KVR + 128
                blocks = [[(c0, min(WB, NKV - c0))] for c0 in range(0, NKV, WB)]
                linear_fm("wdkv%d" % j, DC, blocks, h_in, [hT], tiles, c_kv, wring, p0)
                rstd_from_bank(B_SS, TP, KVR, rkv)
                sts = subtiles(tiles)
                for si, (o, s) in enumerate(sts):
                    lo = o - p0
                    for m in range(KVC):
                        P.op("pe", (lambda e, out=B_TM.ap[0:s, si:si + 1], a=cksq.ap[:, m, lo:lo + s], st=(m == 0), sp=(m == KVC - 1): e.matmul(out, a, ones_f.ap[:, 0:1], start=st, stop=sp)),
                             [cksq.tok, ones_f.tok], [B_TM.tok])
                nst = len(sts)
                ev("dve", (lambda e, o_=rkvc.ap[:, 0:nst], i=B_TM.ap[:, 0:nst]: e.tensor_scalar(o_, i, 1.0 / KVR, EPS, ALU.mult, ALU.add)), [B_TM], [rkvc])
                ev("act", (lambda e, o_=rkvc.ap[:, 0:nst]: e.activation(out=o_, in_=o_, func=AF.Sqrt)), [rkvc], [rkvc])
                ev("dve", (lambda e, o_=rkvc.ap[:, 0:nst]: e.reciprocal(o_, o_)), [rkvc], [rkvc])
                ck_in = lambda kc, o, s: ckT.ap[:, kc, o:o + s]

                def c_k(bi, mi, tile, pb):
                    o, s, cls = tile
                    m = bi * (WB // 128) + mi
                    lo = o - p0
                    sb = stg.next()
                    ev("dve", (lambda e, out=sb.ap[:, 0:s], i=pb.ap[:, 0:s], r=rkv.ap[:, lo:lo + s]: e.tensor_tensor(out, i, r, ALU.mult)), [pb, rkv], [sb])
                    store(KTl[m * 128:(m + 1) * 128, o:o + s], sb, sb.ap[:, 0:s])
                blocks = [[(c0, WB)] for c0 in range(0, H * 128, WB)]
                linear_fm("wukv%d" % j, KVC, blocks, ck_in, [ckT, rkv], tiles, c_k, wring, p0)
                def c_v(bi, st_, pb, ncol):
                    o, s = st_
                    si = sts.index(st_)
                    sb = stg.next()
                    ev("dve", (lambda e, out=sb.ap[0:s, 0:ncol], i=pb.ap[0:s, 0:ncol], r=rkvc.ap[0:s, si:si + 1]: e.tensor_scalar(out, i, r, None, ALU.mult)), [pb, rkvc], [sb])
                    store(Vl[o:o + s, bi * WB:bi * WB + ncol], sb, sb.ap[0:s, 0:ncol])
                blocks = [[(H * 128 + c0, WB)] for c0 in range(0, H * 128, WB)]
                linear_tm("wukv%d" % j, KVC, blocks, ck_in, [ckT, rkvc], sts, c_v, wring, p0)
            else:
                def c_qk(base):
                    def f(bi, mi, tile, pb):
                        o, s, cls = tile
                        m = bi * (WB // 128) + mi
                        dst = (QT if base == 0 else KTl)[m * 128:(m + 1) * 128, o:o + s]
                        rope_store(pb, o, s, p0, rtab, 1, None, stg, tmpring, dst)
                    return f
                blocks = [[(c0, WB)] for c0 in range(0, D, WB)]
                linear_fm("wqkv%d" % j, DC, blocks, h_in, [hT], tiles, c_qk(0), wring, p0)
                blocks = [[(D + c0, WB)] for c0 in range(0, D, WB)]
                linear_fm("wqkv%d" % j, DC, blocks, h_in, [hT], tiles, c_qk(1), wring, p0)
                sts = subtiles(tiles)

                def c_v(bi, st_, pb, ncol):
                    o, s = st_
                    sb = stg.next()
                    ev("act", (lambda e, out=sb.ap[0:s, 0:ncol], i=pb.ap[0:s, 0:ncol]: e.activation(out=out, in_=i, func=AF.Copy)), [pb], [sb])
                    store(Vl[o:o + s, bi * WB:bi * WB + ncol], sb, sb.ap[0:s, 0:ncol])
                blocks = [[(2 * D + c0, WB)] for c0 in range(0, D, WB)]
                linear_tm("wqkv%d" % j, DC, blocks, h_in, [hT], sts, c_v, wring, p0)
        ar.release()
        P.barrier()
        tk, tv = Tok("ktg"), Tok("vg")
        grp = QUADS
        nkc = (H + 1) if mla else 2 * H2
        for kc in range(nkc):
            P.coll("cc", grp, KTl[kc * 128:(kc + 1) * 128, :].opt(), KTg[kc * G * 128:(kc + 1) * G * 128, :].opt(), writes=[tk])
        for jv in range(T // 64):
            P.coll("cc", grp, Vl[jv * 64:(jv + 1) * 64, :].opt(), Vg[jv * G * 64:(jv + 1) * G * 64, :].opt(), writes=[tv])
        P.barrier()
        return nkc

    def phase_B(l, nkc):
        mla = (l % 2 == 0)
        j = l // 2
        ar.mark()
        scale = (192.0 ** -0.5) if mla else (128.0 ** -0.5)
        li = 0.8 - 0.6 * math.exp(-0.3 * l)
        KTv = [KTg[kc_ * G * 128:(kc_ + 1) * G * 128, :].rearrange("(g p) t -> p g t", p=128) for kc_ in range(nkc)]
        Vv = Vg.rearrange("(j g q) d -> q j g d", g=G, q=64)
        ktr = Ring([ar.buf((G, T), BF16, "kt%d" % i) for i in range(2 if mla else 4)])
        ndv = 1 if mla else 2
        NKT = 1 + LT // 128
        vtr = Ring([ar.buf((G, NKT, ndv * 128), BF16, "vt%d" % i) for i in range(2)])
        qtr = Ring([ar.buf((T,), BF16, "qt%d" % i) for i in range(4)])
        ptr = Ring([ar.buf((512,), BF16, "pT%d" % i) for i in range(6)])
        rlr = Ring([ar.buf((512,), F32, "rl%d" % i) for i in range(2)])
        accr = Ring([ar.buf((512,), F32, "acc%d" % i) for i in range(2)])
        ostg = Ring([ar.buf((512,), BF16, "os%d" % i) for i in range(4)])
        of = Ring([ar.buf((2, 512), F32, "of%d" % i) for i in range(2)])
        sq2 = Ring([ar.buf((512,), F32, "sq2_%d" % i) for i in range(2)])
        rs2 = ar.buf((512,), F32, "rs2")
        if mla:
            sring, oring, lring = Ring(banks[0:4]), Ring(banks[4:6]), Ring(banks[6:8])
        else:
            sring, oring, lring = Ring(banks[0:3]), Ring(banks[3:7]), Ring(banks[7:8])
        if mla:
            krope = ar.buf((G, T), BF16, "krope")
            P.dma("sp", "c_kr", krope.ap, KTv[H], writes=[krope.tok])
        else:
            lamt = ar.buf((4, 128), F32, "lamt")
            lt2 = ar.buf((2, 128), F32, "lt2")
            lsum = ar.buf((4,), F32, "lsum")
            sln = ar.buf((2,), F32, "sln")
            P.dma("sp", "c_lam", lamt.ap, small_in["lam%d" % j], writes=[lamt.tok])
            P.dma("sp", "c_sln", sln.ap, small_in["sln%d" % j], writes=[sln.tok])
            ev("dve", lambda e: e.tensor_tensor(lt2.ap[:, 0, :], lamt.ap[:, 0, :], lamt.ap[:, 1, :], ALU.mult), [lamt], [lt2])
            ev("dve", lambda e: e.tensor_tensor(lt2.ap[:, 1, :], lamt.ap[:, 2, :], lamt.ap[:, 3, :], ALU.mult), [lamt], [lt2])
            ev("dve", lambda e: e.reduce_sum(lsum.ap[:, 0:2], lt2.ap, AX.X), [lt2], [lsum])
            ev("act", lambda e: e.activation(out=lsum.ap[:, 0:2], in_=lsum.ap[:, 0:2], func=AF.Exp), [lsum], [lsum])
            ev("dve", lambda e: e.tensor_tensor(lsum.ap[:, 2:3], lsum.ap[:, 0:1], lsum.ap[:, 1:2], ALU.subtract), [lsum], [lsum])
            ev("dve", lambda e: e.tensor_scalar(lsum.ap[:, 2:3], lsum.ap[:, 2:3], float(li), None, ALU.add), [lsum], [lsum])
            ev("dve", lambda e: e.tensor_scalar(sln.ap, sln.ap, float(1.0 - li), None, ALU.mult), [sln], [sln])
        ktiles_all = []
        for g in range(G):
            ktiles_all.append((g, 0, 0, CT))
            for i in range(LT // 128):
                ktiles_all.append((g, 1 + i, CT + i * 128, 128))
        ktiles_ctx = [(g, 0, 0, CT) for g in range(G)]
        qblocks = [(0, CT, ktiles_ctx)]
        QB = 512
        for o in range(CT, T, QB):
            qblocks.append((o, min(QB, T - o), ktiles_all))
        nheads = H if mla else H2
        for h in range(nheads):
            vt = vtr.next()
            vs = "v%d" % ((vtr.i - 1) % 2)
            c0 = h * ndv * 128
            for g in range(G):
                P.dma("sp", vs, vt.ap[0:CT, g, 0, :], Vv[:, 0, g, c0:c0 + ndv * 128], writes=[vt.tok])
                for a_ in range(2):
                    P.dma("sp", vs, vt.ap[a_ * 64:(a_ + 1) * 64, g, 1:NKT, :], Vv[:, 1 + a_::2, g, c0:c0 + ndv * 128], writes=[vt.tok])
            maps = []
            for n in range(1 if mla else 2):
                kc = h if mla else 2 * h + n
                kt = ktr.next()
                P.dma("sp", "k%d" % ((ktr.i - 1) % len(ktr.bufs)), kt.ap, KTv[kc], writes=[kt.tok])
                qt = qtr.next()
                P.dma("sp", "q%d" % ((qtr.i - 1) % 4), qt.ap, QT[kc * 128:(kc + 1) * 128, :], writes=[qt.tok])
                parts = [(kt, qt, 0, 128)]
                if mla:
                    qr = qtr.next()
                    rc = H + h // 2
                    P.dma("sp", "q%d" % ((qtr.i - 1) % 4), qr.ap, QT[rc * 128:(rc + 1) * 128, :], writes=[qr.tok])
                    plo = (h % 2) * 64
                    parts.append((krope, qr, plo, plo + 64))
                maps.append(parts)
            for (qo, qs, ktl) in qblocks:
                res = []
                for n, parts in enumerate(maps):
                    obs = [oring.next() for _ in range(ndv)]
                    lb = lring.next()
                    nk = len(ktl)
                    acc = accr.next()
                    ev("pool", (lambda e, out=acc.ap[:, 0:qs]: e.memset(out, 0.0)), [], [acc])
                    LOOK = (3 if mla else 2)
                    pend = []
                    for kstep in range(nk + LOOK):
                        if kstep < nk:
                            (g, vi, ko, ks) = ktl[kstep]
                            sb_ = sring.next()
                            for pi, (kt, qt, plo, phi) in enumerate(parts):
                                P.op("pe", (lambda e, out=sb_.ap[0:ks, 0:qs], k_=kt.ap[plo:phi, g, ko:ko + ks], q_=qt.ap[plo:phi, qo:qo + qs], st=(pi == 0), sp=(pi == len(parts) - 1):
                                            e.matmul(out, k_, q_, start=st, stop=sp)),
                                     [kt.tok, qt.tok], [sb_.tok])
                            pT = ptr.next()
                            ev("act", (lambda e, out=pT.ap[0:ks, 0:qs], i=sb_.ap[0:ks, 0:qs]: e.activation(out=out, in_=i, func=AF.Exp, scale=float(scale))), [sb_], [pT])
                            ev("dve", (lambda e, a_=acc.ap[0:ks, 0:qs], p_=pT.ap[0:ks, 0:qs]: e.tensor_tensor(a_, a_, p_, ALU.add)), [acc, pT], [acc])
                            pend.append((kstep, g, vi, ks, pT))
                        if kstep >= LOOK:
                            (ki, g, vi, ks, pT) = pend.pop(0)
                            for dv in range(ndv):
                                P.op("pe", (lambda e, out=obs[dv].ap[:, 0:qs], v_=vt.ap[0:ks, g, vi, dv * 128:(dv + 1) * 128], p_=pT.ap[0:ks, 0:qs], st=(ki == 0), sp=(ki == nk - 1):
                                            e.matmul(out, v_, p_, start=st, stop=sp)),
                                     [vt.tok, pT.tok], [obs[dv].tok])
                    P.op("pe", (lambda e, out=lb.ap[:, 0:qs], a_=acc.ap[:, 0:qs]: e.matmul(out, ones_f.ap, a_, start=True, stop=True)),
                         [ones_f.tok, acc.tok], [lb.tok])
                    rl = rlr.next()
                    ev("dve", (lambda e, out=rl.ap[:, 0:qs], i=lb.ap[:, 0:qs]: e.reciprocal(out, i)), [lb], [rl])
                    res.append((obs, rl))
                if mla:
                    obs, rl = res[0]
                    sb = ostg.next()
                    ev("dve", (lambda e, out=sb.ap[:, 0:qs], i=obs[0].ap[:, 0:qs], r=rl.ap[:, 0:qs]: e.tensor_tensor(out, i, r, ALU.mult)), [obs[0], rl], [sb])
                    store(OT[h * 128:(h + 1) * 128, qo:qo + qs], sb, sb.ap[:, 0:qs])
                else:
                    (ob1, rl1), (ob2, rl2) = res
                    ev("dve", (lambda e, out=rl2.ap[:, 0:qs], lam=lsum.ap[:, 2:3]: e.tensor_scalar(out, out, lam, None, ALU.mult)), [rl2, lsum], [rl2])
                    ofb = of.next()
                    for dv in range(2):
                        sq = sq2.next()
                        ev("dve", (lambda e, out=ofb.ap[:, dv, 0:qs], i=ob1[dv].ap[:, 0:qs], r=rl1.ap[:, 0:qs]: e.tensor_tensor(out, i, r, ALU.mult)), [ob1[dv], rl1], [ofb])
                        ev("dve", (lambda e, out=sq.ap[:, 0:qs], i=ob2[dv].ap[:, 0:qs], r=rl2.ap[:, 0:qs]: e.tensor_tensor(out, i, r, ALU.mult)), [ob2[dv], rl2], [sq])
                        ev("pool", (lambda e, out=ofb.ap[:, dv, 0:qs], b=sq.ap[:, 0:qs]: e.tensor_tensor(out, out, b, ALU.subtract)), [ofb, sq], [ofb])
                        ev("act", (lambda e, out=sq.ap[:, 0:qs], i=ofb.ap[:, dv, 0:qs]: e.activation(out=out, in_=i, func=AF.Square)), [ofb], [sq])
                        ssb = ob1[0]
                        P.op("pe", (lambda e, out=ssb.ap[:, 0:qs], r=sq.ap[:, 0:qs], st=(dv == 0), sp=(dv == 1): e.matmul(out, ones_f.ap, r, start=st, stop=sp)),
                             [sq.tok, ones_f.tok], [ssb.tok])
                    ev("dve", (lambda e, out=rs2.ap[:, 0:qs], i=ob1[0].ap[:, 0:qs]: e.tensor_scalar(out, i, 1.0 / 256, EPS, ALU.mult, ALU.add)), [ob1[0]], [rs2])
                    ev("act", (lambda e, out=rs2.ap[:, 0:qs]: e.activation(out=out, in_=out, func=AF.Sqrt)), [rs2], [rs2])
                    ev("dve", (lambda e, out=rs2.ap[:, 0:qs]: e.reciprocal(out, out)), [rs2], [rs2])
                    for dv in range(2):
                        sb = ostg.next()
                        ev("dve", (lambda e, out=sb.ap[:, 0:qs], i=ofb.ap[:, dv, 0:qs], g_=sln.ap[:, dv:dv + 1], r=rs2.ap[:, 0:qs]:
                                   e.scalar_tensor_tensor(out, i, g_, r, ALU.mult, ALU.mult)), [ofb, sln, rs2], [sb])
                        store(OT[(2 * h + dv) * 128:(2 * h + dv + 1) * 128, qo:qo + qs], sb, sb.ap[:, 0:qs])
        ar.release()
        P.barrier()

    def phase_C(l, xsrc, xdst_final, last_layer):
        mla = (l % 2 == 0)
        j = l // 2
        moe = not mla
        ar.mark()
        oT = ar.buf((DC, TPMAX), BF16, "oT")
        yT = ar.buf((DC, TPMAX), F32, "yT")
        EDl = ED if moe else F
        EC = EDl // 128
        aT = ar.buf((EC, TPMAX), BF16, "aT")
        wring = Ring([ar.buf((KCMAX, WB), BF16, "wb%d" % i) for i in range(2)])
        xring = Ring([ar.buf((TPMAX,), F32, "xb%d" % i) for i in range(3)])
        sqring = Ring([ar.buf((TPMAX,), F32, "sq%d" % i) for i in range(2)])
        tmpring = Ring([ar.buf((TPMAX,), F32, "tmp%d" % i) for i in range(3)])
        sgr = Ring([ar.buf((TPMAX,), F32, "sg%d" % i) for i in range(2)])
        rstd = ar.buf((TPMAX,), F32, "rstd")
        if moe:
            comb = ar.buf((E, TPMAX), F32, "comb")
            lg = ar.buf((E, TPMAX), F32, "lg")
            lgs = ar.buf((TPMAX,), F32, "lgs", parts=E)
            m1 = ar.buf((TPMAX,), F32, "m1")
            m2 = ar.buf((TPMAX,), F32, "m2")
            w1 = ar.buf((TPMAX,), F32, "w1")
            wr = ar.buf((DC, E), F32, "wr")
            selm = ar.buf((E, 128), F32, "selm", parts=E)
            P.dma("sp", "c_wr", wr.ap, small_in["wr%d" % j], writes=[wr.tok])
            P.dma("sp", "c_selm", selm.ap.rearrange("p e q -> p (e q)"), selm_in, writes=[selm.tok])
        for tiles in PASSES:
            p0 = tiles[0][0]
            TP = sum(s for (_, s, _) in tiles)
            for c0 in range(0, DC, 8):
                c1 = min(DC, c0 + 8)
                P.dma("sp", "c_ot", oT.ap[:, c0:c1, 0:TP], OT[c0 * 128:c1 * 128, p0:p0 + TP].rearrange("(c p) t -> p c t", p=128), writes=[oT.tok])
            o_in = lambda kc, o, s: oT.ap[:, kc, o:o + s]

            def c_y(bi, mi, tile, pb):
                o, s, cls = tile
                m = bi * (WB // 128) + mi
                lo = o - p0
                ev("dve", (lambda e, out=yT.ap[:, m, lo:lo + s], i=pb.ap[:, 0:s]: e.tensor_copy(out, i)), [pb], [yT])
                sq = sqring.next()
                ev("act", (lambda e, out=sq.ap[:, 0:s], i=pb.ap[:, 0:s]: e.activation(out=out, in_=i, func=AF.Square)), [pb], [sq])
                P.op("pe", (lambda e, out=B_SS.ap[:, lo:lo + s], r=sq.ap[:, 0:s], st=(m == 0 and lo == 0), sp=(m == DC - 1): e.matmul(out, ones_f.ap, r, start=st, stop=sp, skip_group_check=True)),
                     [sq.tok, ones_f.tok], [B_SS.tok])
            blocks = [[(c0, WB)] for c0 in range(0, D, WB)]
            linear_fm("wom%d" % j if mla else "wod%d" % j, DC, blocks, o_in, [oT], tiles, c_y, wring, p0)
            rstd_from_bank(B_SS, TP, D, rstd)
            for c in range(DC):
                xb = xring.next()
                P.dma("sp", "x%d" % ((xring.i - 1) % len(xring.bufs)), xb.ap[:, 0:TP], xsrc[c * 128:(c + 1) * 128, p0:p0 + TP], reads=[txc[c]], writes=[xb.tok])
                for (o, s, cls) in tiles:
                    lo = o - p0
                    ev("dve", (lambda e, out=yT.ap[:, c, lo:lo + s], g_=vcol(cls, l, 2, c), r=rstd.ap[:, lo:lo + s]:
                               e.scalar_tensor_tensor(out, out, g_, r, ALU.mult, ALU.mult)), [yT, vecs, rstd], [yT])
                ev("pool", (lambda e, out=yT.ap[:, c, 0:TP], x=xb.ap[:, 0:TP]: e.tensor_tensor(out, out, x, ALU.add)), [yT, xb], [yT])
                P.dma("pool", "s_yT", xcur[c * 128:(c + 1) * 128, p0:p0 + TP], yT.ap[:, c, 0:TP], reads=[yT.tok], writes=[txc[c]])
                ss_accum(yT.ap[:, c, 0:TP], [yT], TP, 0, c == 0, c == DC - 1, sqring)
            rstd_from_bank(B_SS, TP, D, rstd)
            for c in range(DC):
                tb = tmpring.next()
                for (o, s, cls) in tiles:
                    lo = o - p0
                    ev("dve", (lambda e, out=tb.ap[:, lo:lo + s], x=yT.ap[:, c, lo:lo + s], a=vcol(cls, l, 3, c), r=rstd.ap[:, lo:lo + s]:
                               e.scalar_tensor_tensor(out, x, a, r, ALU.mult, ALU.mult)), [yT, vecs, rstd], [tb])
                    ev("pool", (lambda e, out=tb.ap[:, lo:lo + s], b=vcol(cls, l, 4, c): e.tensor_scalar(out, out, b, None, ALU.add)), [tb, vecs], [tb])
                if moe:
                    P.op("pe", (lambda e, out=B_RT.ap[0:E, 0:TP], w=wr.ap[:, c, :], r=tb.ap[:, 0:TP], st=(c == 0), sp=(c == DC - 1): e.matmul(out, w, r, start=st, stop=sp)),
                         [wr.tok, tb.tok], [B_RT.tok])
                ev("act", (lambda e, out=oT.ap[:, c, 0:TP], i=tb.ap[:, 0:TP]: e.activation(out=out, in_=i, func=AF.Copy)), [tb], [oT])
            h_in = lambda kc, o, s: oT.ap[:, kc, o:o + s]
            if moe:
                ev("act", (lambda e, out=lgs.ap[:, 0:TP], i=B_RT.ap[0:E, 0:TP]: e.activation(out=out, in_=i, func=AF.Copy)), [B_RT], [lgs])
                for e_ in range(E):
                    pb = mmring.next()
                    P.op("pe", (lambda e, out=pb.ap[:, 0:TP], w=selm.ap[:, e_, :], r=lgs.ap[:, 0:TP]: e.matmul(out, w, r, start=True, stop=True)),
                         [selm.tok, lgs.tok], [pb.tok])
                    ev("act", (lambda e, out=lg.ap[:, e_, 0:TP], i=pb.ap[:, 0:TP]: e.activation(out=out, in_=i, func=AF.Copy)), [pb], [lg])
                ev("dve", lambda e: e.tensor_tensor(m1.ap[:, 0:TP], lg.ap[:, 0, 0:TP], lg.ap[:, 1, 0:TP], ALU.max), [lg], [m1])
                for e_ in range(2, E):
                    ev("dve", (lambda e, i=lg.ap[:, e_, 0:TP]: e.tensor_tensor(m1.ap[:, 0:TP], m1.ap[:, 0:TP], i, ALU.max)), [lg, m1], [m1])
                for e_ in range(E):
                    ev("dve", (lambda e, out=comb.ap[:, e_, 0:TP], i=lg.ap[:, e_, 0:TP]: e.tensor_tensor(out, i, m1.ap[:, 0:TP], ALU.is_equal)), [lg, m1], [comb])
                    ev("dve", (lambda e, out=lg.ap[:, e_, 0:TP], mk=comb.ap[:, e_, 0:TP]: e.scalar_tensor_tensor(out, mk, -1e30, out, ALU.mult, ALU.add)), [lg, comb], [lg])
                ev("dve", lambda e: e.tensor_tensor(m2.ap[:, 0:TP], lg.ap[:, 0, 0:TP], lg.ap[:, 1, 0:TP], ALU.max), [lg], [m2])
                for e_ in range(2, E):
                    ev("dve", (lambda e, i=lg.ap[:, e_, 0:TP]: e.tensor_tensor(m2.ap[:, 0:TP], m2.ap[:, 0:TP], i, ALU.max)), [lg, m2], [m2])
                ev("dve", lambda e: e.tensor_tensor(w1.ap[:, 0:TP], m1.ap[:, 0:TP], m2.ap[:, 0:TP], ALU.subtract), [m1, m2], [w1])
                ev("act", lambda e: e.activation(out=w1.ap[:, 0:TP], in_=w1.ap[:, 0:TP], func=AF.Sigmoid), [w1], [w1])
                ev("dve", lambda e: e.tensor_scalar(m1.ap[:, 0:TP], w1.ap[:, 0:TP], -1.0, 1.0, ALU.mult, ALU.add), [w1], [m1])
                for e_ in range(E):
                    tb = tmpring.next()
                    ev("dve", (lambda e, out=tb.ap[:, 0:TP], i=lg.ap[:, e_, 0:TP]: e.tensor_tensor(out, i, m2.ap[:, 0:TP], ALU.is_equal)), [lg, m2], [tb])
                    ev("dve", (lambda e, out=tb.ap[:, 0:TP]: e.tensor_tensor(out, out, m1.ap[:, 0:TP], ALU.mult)), [tb, m1], [tb])
                    ev("dve", (lambda e, out=comb.ap[:, e_, 0:TP]: e.tensor_tensor(out, out, w1.ap[:, 0:TP], ALU.mult)), [comb, w1], [comb])
                    ev("dve", (lambda e, out=comb.ap[:, e_, 0:TP], t_=tb.ap[:, 0:TP]: e.tensor_tensor(out, out, t_, ALU.add)), [comb, tb], [comb])
            for ex in range(E if moe else 1):
                gname = ("mgu%d_%d" % (j, ex)) if moe else ("wgu%d" % j)
                dname = ("mdn%d_%d" % (j, ex)) if moe else ("wdn%d" % j)

                def c_gu(bi, mi, tile, pb):
                    o, s, cls = tile
                    lo = o - p0
                    if mi == 0:
                        sg = sgr.bufs[bi % 2]
                        ev("act", (lambda e, out=sg.ap[:, lo:lo + s], i=pb.ap[:, 0:s]: e.activation(out=out, in_=i, func=AF.Silu)), [pb], [sg])
                    else:
                        sg = sgr.bufs[bi % 2]
                        if moe:
                            ev("dve", (lambda e, out=sg.ap[:, lo:lo + s], i=pb.ap[:, 0:s]: e.tensor_tensor(out, out, i, ALU.mult)), [pb, sg], [sg])
                            ev("pool", (lambda e, out=aT.ap[:, bi, lo:lo + s], a=sg.ap[:, lo:lo + s], c_=comb.ap[:, ex, lo:lo + s]: e.tensor_tensor(out, a, c_, ALU.mult)), [sg, comb], [aT])
                        else:
                            ev("dve", (lambda e, out=aT.ap[:, bi, lo:lo + s], a=sg.ap[:, lo:lo + s], i=pb.ap[:, 0:s]: e.tensor_tensor(out, a, i, ALU.mult)), [pb, sg], [aT])
                blocks = [[(m * 128, 128), (EDl + m * 128, 128)] for m in range(EC)]
                linear_fm(gname, DC, blocks, h_in, [oT], tiles, c_gu, wring, p0)
                a_in = lambda kc, o, s: aT.ap[:, kc, o:o + s]

                def c_dn(bi, mi, tile, pb):
                    o, s, cls = tile
                    m = bi * (WB // 128) + mi
                    lo = o - p0
                    if ex == 0:
                        ev("act", (lambda e, out=yT.ap[:, m, lo:lo + s], i=pb.ap[:, 0:s]: e.activation(out=out, in_=i, func=AF.Copy)), [pb], [yT])
                    else:
                        ev("dve", (lambda e, out=yT.ap[:, m, lo:lo + s], i=pb.ap[:, 0:s]: e.tensor_tensor(out, out, i, ALU.add)), [pb, yT], [yT])
                blocks = [[(c0, WB)] for c0 in range(0, D, WB)]
                linear_fm(dname, EC, blocks, a_in, [aT], tiles, c_dn, wring, p0)
            for c in range(DC):
                ss_accum(yT.ap[:, c, 0:TP], [yT], TP, 0, c == 0, c == DC - 1, sqring)
            rstd_from_bank(B_SS, TP, D, rstd)
            for c in range(DC):
                xb = xring.next()
                P.dma("sp", "x%d" % ((xring.i - 1) % len(xring.bufs)), xb.ap[:, 0:TP], xcur[c * 128:(c + 1) * 128, p0:p0 + TP], reads=[txc[c]], writes=[xb.tok])
                for (o, s, cls) in tiles:
                    lo = o - p0
                    ev("dve", (lambda e, out=yT.ap[:, c, lo:lo + s], g_=vcol(cls, l, 5, c), r=rstd.ap[:, lo:lo + s]:
                               e.scalar_tensor_tensor(out, out, g_, r, ALU.mult, ALU.mult)), [yT, vecs, rstd], [yT])
                ev("pool", (lambda e, out=yT.ap[:, c, 0:TP], x=xb.ap[:, 0:TP]: e.tensor_tensor(out, out, x, ALU.add)), [yT, xb], [yT])
                P.dma("pool", "s_yT", xdst_final[c * 128:(c + 1) * 128, p0:p0 + TP], yT.ap[:, c, 0:TP], reads=[yT.tok], writes=([txc[c]] if not last_layer else []))
        ar.release()
        P.barrier()

    K.phase_A, K.phase_B, K.phase_C = phase_A, phase_B, phase_C
    xsrc = xT_in
    for l in range(nlayers):
        nkc = phase_A(l, xsrc)
        if stop == "A%d" % l:
            break
        phase_B(l, nkc)
        if stop == "B%d" % l:
            break
        last = (l == nlayers - 1)
        phase_C(l, xsrc, yT_out if last else xcur, last)
        xsrc = xcur
    for name in dbg:
        if name == "QT":
            P.dma("pool", "dbg", dbg[name], QT)
        if name == "KTg":
            P.dma("pool", "dbg", dbg[name], KTg)
        if name == "Vg":
            P.dma("pool", "dbg", dbg[name], Vg)
        if name == "OT":
            P.dma("pool", "dbg", dbg[name], OT)
    fin()
    K.arena_peak = ar.peak
    K.nops = {e: len(P.ops[e]) for e in ENGS}
    return nc


def run(cfg, inp, nlayers=4, debug=(), trace=False):
    maps = prepare_inputs(cfg, inp)
    nc = build(cfg, nlayers, debug)
    res = run_bass_kernel_spmd(nc, maps, core_ids=list(range(8)), trace=trace)
    return res


def assemble(cfg, res):
    B, G, LT, CT, D = cfg["B"], cfg["G"], cfg["LT"], cfg["CT"], cfg["D"]
    out = np.empty((B, G * LT, D), np.float32)
    for c in range(8):
        b, r = c // G, c % G
        yT = np.asarray(res.results[c]["yT"])
        out[b, r * LT:(r + 1) * LT] = yT[:, CT:].T
    return out


_CFG = make_cfg(False)


def kernel(**inputs):
    res = run(_CFG, inputs)
    return assemble(_CFG, res)
```
